# Optimizing a Trainium2 kernel written in Bass

```python
import math
import jax
import jax.numpy as jnp
from jax import lax
import numpy as np

D_MODEL = 2048
BATCH = 4
SEQ = 2048
DEPTH = 4

GRID_W = 64
CTX_LEN = 256
N_MIXERS = 3
HEAD_DIM = 128
ROPE_THETA = 10000.0
Q_BLOCK = 128
NORM_EPS = 1e-6
MASK_VALUE = -1e30

NA_HEADS = D_MODEL // HEAD_DIM
NA_WIN_H = 8
NA_WIN_W = 16

GQA_HEADS = D_MODEL // HEAD_DIM
GQA_KV_HEADS = GQA_HEADS // 4

DIFF_HEADS = D_MODEL // (2 * HEAD_DIM)
DIFF_LAMBDA_STD = 0.1

N_EXPERTS = 32
TOP_K = 4
D_EXPERT = 3 * D_MODEL // 8
SWIGLU_LIMIT = 7.0
SWIGLU_ALPHA = 1.702
MOE_BLOCK = 128

N_NA_LAYERS = len(range(0, DEPTH, N_MIXERS))
N_GQA_LAYERS = len(range(1, DEPTH, N_MIXERS))
N_DIFF_LAYERS = len(range(2, DEPTH, N_MIXERS))

kernel_name = 'hybrid_natten_gqa_diffattn_moe_dit'


def rms_norm(x, g):
    xf = x.astype(jnp.float32)
    y = xf * lax.rsqrt(jnp.mean(xf * xf, axis=-1, keepdims=True) + NORM_EPS)
    return (y * g.astype(jnp.float32)).astype(x.dtype)


def modulate(h, shift, scale):
    return h * (1.0 + scale) + shift


def softmax_f32(s):
    return jax.nn.softmax(s.astype(jnp.float32), axis=-1)


def axial_rope_tables(n_tok, dtype):
    t = jnp.arange(n_tok)
    row = (t // GRID_W).astype(jnp.float32)
    col = (t % GRID_W).astype(jnp.float32)
    half = HEAD_DIM // 2
    inv_freq = ROPE_THETA ** (-jnp.arange(0, half, 2, dtype=jnp.float32) / half)
    ang_r = row[:, None] * inv_freq[None, :]
    ang_c = col[:, None] * inv_freq[None, :]
    ang = jnp.concatenate([ang_r, ang_r, ang_c, ang_c], axis=-1)
    return jnp.cos(ang).astype(dtype), jnp.sin(ang).astype(dtype)


def rotate_half(u):
    h = u.shape[-1] // 2
    return jnp.concatenate([-u[..., h:], u[..., :h]], axis=-1)


def apply_axial_rope(x, cos, sin):
    half = x.shape[-1] // 2
    rot = jnp.concatenate([rotate_half(x[..., :half]), rotate_half(x[..., half:])], axis=-1)
    return x * cos + rot * sin


def sweep_query_blocks(fn, q):
    B, H, S, Dh = q.shape
    qb = q.reshape(B, H, S // Q_BLOCK, Q_BLOCK, Dh).transpose(2, 0, 1, 3, 4)
    out = lax.map(fn, qb)
    nb, _, Ho, blk, Dv = out.shape
    return out.transpose(1, 2, 0, 3, 4).reshape(B, Ho, nb * blk, Dv)


def heads_out(o, w_o):
    B, H, T, Dv = o.shape
    return o.transpose(0, 2, 1, 3).reshape(B, T, H * Dv) @ w_o


def project_out(o_ctx, o_lat, w_o, need_ctx):
    if need_ctx:
        L = o_ctx.shape[2]
        y = heads_out(jnp.concatenate([o_ctx, o_lat], axis=2), w_o)
        return y[:, :L], y[:, L:]
    return None, heads_out(o_lat, w_o)


def neighbourhood_attention(h_ctx, h_lat, w_qkv, w_o, rpb, need_ctx):
    B, S, _ = h_lat.shape
    L = h_ctx.shape[1]
    rows = S // GRID_W
    wh = min(NA_WIN_H, rows)
    scale = HEAD_DIM ** -0.5
    qkv = (jnp.concatenate([h_ctx, h_lat], axis=1) @ w_qkv).reshape(B, L + S, 3, NA_HEADS, HEAD_DIM)
    q, k, v = (qkv[:, :, i].transpose(0, 2, 1, 3) for i in range(3))
    kc, vc = k[:, :, :L], v[:, :, :L]
    kg = k[:, :, L:].reshape(B, NA_HEADS, rows, GRID_W, HEAD_DIM)
    vg = v[:, :, L:].reshape(B, NA_HEADS, rows, GRID_W, HEAD_DIM)
    qg = q[:, :, L:].reshape(B, NA_HEADS, rows, GRID_W, HEAD_DIM).transpose(2, 0, 1, 3, 4)
    col = jnp.arange(GRID_W)
    col_start = jnp.clip(col - NA_WIN_W // 2, 0, GRID_W - NA_WIN_W)
    col_mask = (col[None, :] >= col_start[:, None]) & (col[None, :] < col_start[:, None] + NA_WIN_W)
    dc_idx = jnp.clip(col[None, :] - col[:, None] + NA_WIN_W - 1, 0, 2 * NA_WIN_W - 2)

    def row_block(args):
        r, q_r = args
        r0 = jnp.clip(r - wh // 2, 0, rows - wh)
        k_win = lax.dynamic_slice_in_dim(kg, r0, wh, axis=2)
        v_win = lax.dynamic_slice_in_dim(vg, r0, wh, axis=2)
        dr_idx = r0 + jnp.arange(wh) - r + NA_WIN_H - 1
        bias = rpb[:, dr_idx[:, None, None], dc_idx[None, :, :]].transpose(0, 2, 1, 3)
        s_lat = jnp.einsum('bhqd,bhrkd->bhqrk', q_r, k_win).astype(jnp.float32) * scale + bias[None].astype(jnp.float32)
        s_lat = jnp.where(col_mask[:, None, :], s_lat, MASK_VALUE)
        s_ctx = jnp.einsum('bhqd,bhkd->bhqk', q_r, kc).astype(jnp.float32) * scale
        p = softmax_f32(jnp.concatenate([s_ctx, s_lat.reshape(B, NA_HEADS, GRID_W, wh * GRID_W)], axis=-1)).astype(v_win.dtype)
        p_lat = p[..., L:].reshape(B, NA_HEADS, GRID_W, wh, GRID_W)
        return jnp.einsum('bhqk,bhkd->bhqd', p[..., :L], vc) + jnp.einsum('bhqrk,bhrkd->bhqd', p_lat, v_win)

    o_lat = lax.map(row_block, (jnp.arange(rows), qg))
    o_lat = o_lat.transpose(1, 2, 0, 3, 4).reshape(B, NA_HEADS, S, HEAD_DIM)
    o_ctx = None
    if need_ctx:
        p_c = softmax_f32(jnp.einsum('bhqd,bhkd->bhqk', q[:, :, :L], kc).astype(jnp.float32) * scale).astype(vc.dtype)
        o_ctx = jnp.einsum('bhqk,bhkd->bhqd', p_c, vc)
    return project_out(o_ctx, o_lat, w_o, need_ctx)


def gqa_attention(h_ctx, h_lat, w_qkv, w_o, q_g, k_g, cos, sin, need_ctx):
    B, S, D = h_lat.shape
    L = h_ctx.shape[1]
    T = L + S
    G = GQA_HEADS // GQA_KV_HEADS
    kv_dim = GQA_KV_HEADS * HEAD_DIM
    scale = HEAD_DIM ** -0.5
    t = jnp.concatenate([h_ctx, h_lat], axis=1) @ w_qkv
    q = rms_norm(t[..., :D].reshape(B, T, GQA_HEADS, HEAD_DIM), q_g).transpose(0, 2, 1, 3)
    k = rms_norm(t[..., D:D + kv_dim].reshape(B, T, GQA_KV_HEADS, HEAD_DIM), k_g).transpose(0, 2, 1, 3)
    v = t[..., D + kv_dim:].reshape(B, T, GQA_KV_HEADS, HEAD_DIM).transpose(0, 2, 1, 3)
    q_lat = apply_axial_rope(q[:, :, L:], cos, sin)
    k_all = jnp.concatenate([k[:, :, :L], apply_axial_rope(k[:, :, L:], cos, sin)], axis=2)

    def attend(qb, kk, vv):
        n = qb.shape[2]
        qgrp = qb.reshape(B, GQA_KV_HEADS, G, n, HEAD_DIM)
        s = jnp.einsum('bkgqd,bksd->bkgqs', qgrp, kk).astype(jnp.float32) * scale
        p = softmax_f32(s).astype(vv.dtype)
        return jnp.einsum('bkgqs,bksd->bkgqd', p, vv).reshape(B, GQA_HEADS, n, HEAD_DIM)

    o_lat = sweep_query_blocks(lambda qb: attend(qb, k_all, v), q_lat)
    o_ctx = attend(q[:, :, :L], k[:, :, :L], v[:, :, :L]) if need_ctx else None
    return project_out(o_ctx, o_lat, w_o, need_ctx)


def diff_attention(h_ctx, h_lat, w_qkv, w_o, lam, subln_g, cos, sin, layer_idx, need_ctx):
    B, S, D = h_lat.shape
    L = h_ctx.shape[1]
    T = L + S
    scale = HEAD_DIM ** -0.5
    lambda_init = 0.8 - 0.6 * math.exp(-0.3 * layer_idx)
    lam32 = lam.astype(jnp.float32)
    lam_full = jnp.exp(jnp.sum(lam32[0] * lam32[1])) - jnp.exp(jnp.sum(lam32[2] * lam32[3])) + lambda_init
    t = jnp.concatenate([h_ctx, h_lat], axis=1) @ w_qkv
    q = t[..., :D].reshape(B, T, DIFF_HEADS, 2, HEAD_DIM).transpose(0, 2, 3, 1, 4)
    k = t[..., D:2 * D].reshape(B, T, DIFF_HEADS, 2, HEAD_DIM).transpose(0, 2, 3, 1, 4)
    v = t[..., 2 * D:].reshape(B, T, DIFF_HEADS, 2 * HEAD_DIM).transpose(0, 2, 1, 3)
    q_lat = apply_axial_rope(q[:, :, :, L:], cos, sin).reshape(B, 2 * DIFF_HEADS, S, HEAD_DIM)
    k_all = jnp.concatenate([k[:, :, :, :L], apply_axial_rope(k[:, :, :, L:], cos, sin)], axis=3)

    def attend(qb, kk, vv):
        n = qb.shape[2]
        s = jnp.einsum('bhcqd,bhckd->bhcqk', qb.reshape(B, DIFF_HEADS, 2, n, HEAD_DIM), kk).astype(jnp.float32) * scale
        p = softmax_f32(s)
        a = (p[:, :, 0] - lam_full * p[:, :, 1]).astype(vv.dtype)
        return jnp.einsum('bhqk,bhkd->bhqd', a, vv)

    def finish(o):
        return rms_norm(o, subln_g) * (1.0 - lambda_init)

    o_lat = finish(sweep_query_blocks(lambda qb: attend(qb, k_all, v), q_lat))
    o_ctx = None
    if need_ctx:
        q_c = q[:, :, :, :L].reshape(B, 2 * DIFF_HEADS, L, HEAD_DIM)
        o_ctx = finish(attend(q_c, k[:, :, :, :L], v[:, :, :L]))
    return project_out(o_ctx, o_lat, w_o, need_ctx)


def moe_ffn(h, router_w, router_b, w_gate, b_gate, w_up, b_up, w_down, b_down):
    B, T, D = h.shape
    n_tok = B * T
    xf = h.reshape(n_tok, D)
    logits = (xf @ router_w + router_b).astype(jnp.float32)
    top_val, top_idx = lax.top_k(logits, TOP_K)
    gates = jax.nn.softmax(top_val, axis=-1)
    n_assign = n_tok * TOP_K
    e_flat = top_idx.reshape(-1).astype(jnp.int32)
    order = jnp.argsort(e_flat)
    e_sorted = e_flat[order]
    tok_sorted = (order // TOP_K).astype(jnp.int32)
    gate_sorted = gates.reshape(-1)[order]
    counts = jnp.zeros((N_EXPERTS,), jnp.int32).at[e_flat].add(1)
    padded = ((counts + MOE_BLOCK - 1) // MOE_BLOCK) * MOE_BLOCK
    start = jnp.cumsum(counts) - counts
    pend = jnp.cumsum(padded)
    pstart = pend - padded
    dest = pstart[e_sorted] + (jnp.arange(n_assign, dtype=jnp.int32) - start[e_sorted])
    n_blocks = -(-n_assign // MOE_BLOCK) + N_EXPERTS
    n_slots = n_blocks * MOE_BLOCK
    slot_tok = jnp.full((n_slots,), n_tok, jnp.int32).at[dest].set(tok_sorted)
    slot_gate = jnp.zeros((n_slots,), jnp.float32).at[dest].set(gate_sorted)
    block_expert = jnp.clip(jnp.searchsorted(pend, jnp.arange(n_blocks, dtype=jnp.int32) * MOE_BLOCK, side='right'), 0, N_EXPERTS - 1)
    x_pad = jnp.concatenate([xf, jnp.zeros((1, D), xf.dtype)], axis=0)
    x_slots = x_pad[slot_tok].reshape(n_blocks, MOE_BLOCK, D)

    def expert_block(args):
        e, xb = args
        g = jnp.minimum(xb @ w_gate[e] + b_gate[e], SWIGLU_LIMIT)
        u = jnp.clip(xb @ w_up[e] + b_up[e], -SWIGLU_LIMIT, SWIGLU_LIMIT)
        return ((u + 1.0) * (g * jax.nn.sigmoid(SWIGLU_ALPHA * g))) @ w_down[e] + b_down[e]

    y_slots = lax.map(expert_block, (block_expert, x_slots)).reshape(n_slots, D)
    y = jax.ops.segment_sum(y_slots * slot_gate[:, None].astype(y_slots.dtype), slot_tok, num_segments=n_tok + 1)[:n_tok]
    return y.reshape(B, T, D)


def setup_inputs(seed: int = 0) -> dict:
    key = jax.random.key(seed)
    ks = iter(jax.random.split(key, 32))
    D = D_MODEL
    kv_dim = GQA_KV_HEADS * HEAD_DIM

    def nrm(shape, scale):
        return jax.random.normal(next(ks), shape, jnp.float32) * scale

    return {
        'x': nrm((BATCH, SEQ, D), 1.0),
        'c': nrm((BATCH, D), 1.0),
        'ctx': nrm((BATCH, CTX_LEN, D), 1.0),
        'c_ctx': nrm((D,), 1.0),
        'ada_w': nrm((DEPTH, D, 6 * D), 0.5 * D ** -0.5),
        'ada_b': nrm((DEPTH, 6 * D), 0.02),
        'norm_mix_g': 1.0 + nrm((DEPTH, D), 0.02),
        'norm_ffn_g': 1.0 + nrm((DEPTH, D), 0.02),
        'final_g': 1.0 + nrm((D,), 0.02),
        'na_wqkv': nrm((N_NA_LAYERS, D, 3 * D), D ** -0.5),
        'na_wo': nrm((N_NA_LAYERS, D, D), D ** -0.5),
        'na_rpb': nrm((N_NA_LAYERS, NA_HEADS, 2 * NA_WIN_H - 1, 2 * NA_WIN_W - 1), 0.1),
        'gqa_wqkv': nrm((N_GQA_LAYERS, D, D + 2 * kv_dim), D ** -0.5),
        'gqa_wo': nrm((N_GQA_LAYERS, D, D), D ** -0.5),
        'gqa_q_g': 1.0 + nrm((N_GQA_LAYERS, HEAD_DIM), 0.02),
        'gqa_k_g': 1.0 + nrm((N_GQA_LAYERS, HEAD_DIM), 0.02),
        'diff_wqkv': nrm((N_DIFF_LAYERS, D, 3 * D), D ** -0.5),
        'diff_wo': nrm((N_DIFF_LAYERS, D, D), D ** -0.5),
        'diff_lambda': nrm((N_DIFF_LAYERS, 4, HEAD_DIM), DIFF_LAMBDA_STD),
        'diff_subln_g': 1.0 + nrm((N_DIFF_LAYERS, 2 * HEAD_DIM), 0.02),
        'router_w': nrm((DEPTH, D, N_EXPERTS), D ** -0.5),
        'router_b': nrm((DEPTH, N_EXPERTS), 0.01),
        'w_gate': nrm((DEPTH, N_EXPERTS, D, D_EXPERT), D ** -0.5),
        'b_gate': nrm((DEPTH, N_EXPERTS, D_EXPERT), 0.02),
        'w_up': nrm((DEPTH, N_EXPERTS, D, D_EXPERT), D ** -0.5),
        'b_up': nrm((DEPTH, N_EXPERTS, D_EXPERT), 0.02),
        'w_down': nrm((DEPTH, N_EXPERTS, D_EXPERT, D), D_EXPERT ** -0.5),
        'b_down': nrm((DEPTH, N_EXPERTS, D), 0.02),
    }


def reference(x, c, ctx, c_ctx, ada_w, ada_b, norm_mix_g, norm_ffn_g, final_g,
              na_wqkv, na_wo, na_rpb, gqa_wqkv, gqa_wo, gqa_q_g, gqa_k_g,
              diff_wqkv, diff_wo, diff_lambda, diff_subln_g,
              router_w, router_b, w_gate, b_gate, w_up, b_up, w_down, b_down):
    B, S, D = x.shape
    L = ctx.shape[1]
    cos, sin = axial_rope_tables(S, x.dtype)
    silu_c = jax.nn.silu(c)
    silu_cc = jax.nn.silu(c_ctx)
    for i in range(DEPTH):
        need_ctx = i < DEPTH - 1
        mod_lat = jnp.split((silu_c @ ada_w[i] + ada_b[i])[:, None, :], 6, axis=-1)
        mod_ctx = jnp.split((silu_cc @ ada_w[i] + ada_b[i])[None, None, :], 6, axis=-1)
        h_lat = modulate(rms_norm(x, norm_mix_g[i]), mod_lat[0], mod_lat[1])
        h_ctx = modulate(rms_norm(ctx, norm_mix_g[i]), mod_ctx[0], mod_ctx[1])
        kind, j = i % N_MIXERS, i // N_MIXERS
        if kind == 0:
            y_ctx, y_lat = neighbourhood_attention(h_ctx, h_lat, na_wqkv[j], na_wo[j], na_rpb[j], need_ctx)
        elif kind == 1:
            y_ctx, y_lat = gqa_attention(h_ctx, h_lat, gqa_wqkv[j], gqa_wo[j], gqa_q_g[j], gqa_k_g[j], cos, sin, need_ctx)
        else:
            y_ctx, y_lat = diff_attention(h_ctx, h_lat, diff_wqkv[j], diff_wo[j], diff_lambda[j], diff_subln_g[j], cos, sin, i, need_ctx)
        x = x + mod_lat[2] * y_lat
        h_lat = modulate(rms_norm(x, norm_ffn_g[i]), mod_lat[3], mod_lat[4])
        moe_params = (router_w[i], router_b[i], w_gate[i], b_gate[i], w_up[i], b_up[i], w_down[i], b_down[i])
        if need_ctx:
            ctx = ctx + mod_ctx[2] * y_ctx
            h_ctx = modulate(rms_norm(ctx, norm_ffn_g[i]), mod_ctx[3], mod_ctx[4])
            y = moe_ffn(jnp.concatenate([h_ctx, h_lat], axis=1), *moe_params)
            ctx = ctx + mod_ctx[5] * y[:, :L]
            x = x + mod_lat[5] * y[:, L:]
        else:
            x = x + mod_lat[5] * moe_ffn(h_lat, *moe_params)
    return rms_norm(x, final_g)
```

```python
import math
import contextlib
import numpy as np
import ml_dtypes
import concourse.bass as bass
import concourse.mybir as mybir
from concourse.bass_utils import run_bass_kernel_spmd

F32 = mybir.dt.float32
BF16 = mybir.dt.bfloat16
AF = mybir.ActivationFunctionType
ALU = mybir.AluOpType
AX = mybir.AxisListType

ENGS = ("pe", "act", "dve", "pool", "sp")
DMA_SLOTS = 6


class Op:
    __slots__ = ("eng", "fn", "deps", "is_dma", "needs_inc", "token", "idx")

    def __init__(self, eng, fn, is_dma):
        self.eng = eng
        self.fn = fn
        self.deps = []
        self.is_dma = is_dma
        self.needs_inc = is_dma
        self.token = None
        self.idx = -1


class Prog:
    def __init__(self, nc):
        self.nc = nc
        self.eng_ops = {e: [] for e in ENGS}
        self.last_w = {}
        self.readers = {}
        self.dma_count = {e: 0 for e in ENGS}
        self.dma_hist = {e: [] for e in ENGS}
        self.final_wait = []

    def op(self, eng, fn, reads=(), writes=(), dma=False):
        o = Op(eng, fn, dma)
        deps = set()
        for k in reads:
            w = self.last_w.get(k)
            if w is not None:
                deps.add(w)
        for k in writes:
            w = self.last_w.get(k)
            if w is not None:
                deps.add(w)
            for r in self.readers.get(k, ()):
                deps.add(r)
        if dma:
            hist = self.dma_hist[eng]
            if len(hist) >= DMA_SLOTS:
                deps.add(hist[-DMA_SLOTS])
            hist.append(o)
        deps.discard(o)
        for d in deps:
            if d.eng == "pe" and eng == "pe" and not d.is_dma and not dma:
                continue
            o.deps.append(d)
            d.needs_inc = True
        for k in writes:
            self.last_w[k] = o
            self.readers[k] = []
        for k in reads:
            if k in writes:
                continue
            lst = self.readers.setdefault(k, [])
            if not dma:
                lst[:] = [r for r in lst if r.is_dma or r.eng != eng]
            lst.append(o)
        o.idx = len(self.eng_ops[eng])
        self.eng_ops[eng].append(o)
        return o

    def barrier(self):
        lasts = []
        for e in ENGS:
            for o in reversed(self.eng_ops[e]):
                if not o.is_dma and o.fn is not None:
                    lasts.append(o)
                    break
            lasts.extend(self.dma_hist[e][-DMA_SLOTS:])
        for e in ENGS:
            o = Op(e, None, False)
            for d in lasts:
                o.deps.append(d)
                d.needs_inc = True
            self.eng_ops[e].append(o)

    def emit(self, sems, dsems):
        nc = self.nc
        for e in ENGS:
            cnt = 0
            dcnt = 0
            for o in self.eng_ops[e]:
                if o.is_dma:
                    slot = dcnt % DMA_SLOTS
                    o.token = (dsems[e][slot], 16 * (dcnt // DMA_SLOTS + 1))
                    dcnt += 1
                elif o.needs_inc:
                    cnt += 1
                    o.token = (sems[e], cnt)
        all_dma = [o for e in ENGS for o in self.eng_ops[e] if o.is_dma]
        handles = {"pe": nc.tensor, "act": nc.scalar, "dve": nc.vector, "pool": nc.gpsimd, "sp": nc.sync}

        def run_engine(e, eng):
            known = {}
            for o in self.eng_ops[e]:
                waits = {}
                for d in o.deps:
                    s, v = d.token
                    key = id(s)
                    if key not in waits or waits[key][1] < v:
                        waits[key] = (s, v)
                for key, (s, v) in waits.items():
                    if known.get(key, 0) >= v:
                        continue
                    eng.wait_ge(s, v)
                    known[key] = v
                if o.fn is None:
                    continue
                ins = o.fn(eng)
                if o.token is not None:
                    s, v = o.token
                    ins.then_inc(s, 16 if o.is_dma else 1)
            if e == "sp":
                last = {}
                for o in all_dma:
                    s, v = o.token
                    key = id(s)
                    if key not in last or last[key][1] < v:
                        last[key] = (s, v)
                for key, (s, v) in last.items():
                    if known.get(key, 0) < v:
                        eng.wait_ge(s, v)

        with nc.Block() as block:
            @block.tensor
            def _(eng):
                run_engine("pe", eng)

            @block.scalar
            def _(eng):
                run_engine("act", eng)

            @block.vector
            def _(eng):
                run_engine("dve", eng)

            @block.gpsimd
            def _(eng):
                run_engine("pool", eng)

            @block.sync
            def _(eng):
                run_engine("sp", eng)


def build_and_emit(body):
    nc = bass.Bass("TRN2", target_bir_lowering=False)
    with contextlib.ExitStack() as es:
        P = Prog(nc)
        body(nc, P, es)
        sems = {e: es.enter_context(nc.semaphore("s_" + e)) for e in ENGS}
        dsems = {e: [es.enter_context(nc.semaphore("d_%s%d" % (e, i))) for i in range(DMA_SLOTS)]
                 for e in ("sp", "pool", "act")}
        dsems["pe"] = dsems["dve"] = None
        P.emit(sems, dsems)
    return nc


D = 2048
NCH = 16
LCTX = 256
SEQ = 2048
T_ALL = 2304
T_OWN = 1152
NE = 32
DFF = 768
NJ = 6
EPS = 1e-6
SCALE = 128 ** -0.5
KINDS = ("na", "gqa", "diff", "na")
ALL_TILES = [(0, 512, 0), (512, 512, 0), (1024, 512, 0), (1536, 512, 0), (2048, 256, 1)]
OWN_TILES = [(0, 512, 0, 0), (512, 512, 0, 512), (1024, 128, 1, 2048)]


def na_chunk_plan():
    plan = []
    tid = 0
    shared = None
    for pl in range(8):
        lo0, hi0 = max(2 * pl - 4, 0), max(2 * pl - 3, 0) + 7
        lo1, hi1 = min(2 * pl - 4, 8), min(2 * pl - 3, 8) + 7
        lo, hi = min(lo0, lo1), max(hi0, hi1)
        chunks = list(range(lo // 2, hi // 2 + 1))
        if 2 <= pl <= 5:
            if shared is None:
                shared = list(range(tid, tid + len(chunks)))
                tid += len(chunks)
            tids = shared
        else:
            tids = list(range(tid, tid + len(chunks)))
            tid += len(chunks)
        plan.append([(c % 16, c, t) for c, t in zip(chunks, tids)])
    return plan, tid


NA_PLAN, NA_NT = na_chunk_plan()


def kcols(name, c, col0, n):
    return [(name, c, cb) for cb in range(col0 // 128, (col0 + n + 127) // 128)]


def segs(hh, ls, n):
    out = []
    bounds = [(0, 1024, hh * 1024), (1024, 2048, (1 - hh) * 1024),
              (2048, 2176, 2048 + hh * 128), (2176, 2304, 2048 + (1 - hh) * 128)]
    for (a, b, g) in bounds:
        lo, hi = max(ls, a), min(ls + n, b)
        if lo < hi:
            out.append((lo, g + lo - a, hi - lo))
    return out


def xd_keys(buf, gs, n):
    return [("Xd", buf, gb) for gb in range(gs // 128, (gs + n + 127) // 128)]


def fused_body(nc, P, es):
    def din(name, shape, dt=F32):
        return nc.dram_tensor(name, list(shape), dt, kind="ExternalInput").ap()

    uniq = [0]

    def sb(name, shape, dt, stack=None):
        uniq[0] += 1
        return (stack or es).enter_context(nc.sbuf_tensor("%s_%d" % (name, uniq[0]), list(shape), dt))

    x_in = din("x_in", [128, NCH, T_ALL])
    c2T = din("c2T", [128, NCH, 2])
    cst = din("cst", [128, 4, 128])
    fgT = din("fgT", [128, NCH])
    cosT_h = [din("cosT_h%d" % h, [128, SEQ]) for h in range(2)]
    sinT_h = [din("sinT_h%d" % h, [128, SEQ]) for h in range(2)]
    outT_all = nc.dram_tensor("outT", [2, 128, NCH, 1024], F32, kind="ExternalOutput").ap()
    Xs = [nc.dram_tensor("Xs%d" % i, [128, NCH, T_ALL], F32).ap() for i in range(2)]
    q_s = nc.dram_tensor("q_s", [16, 128, T_OWN], BF16).ap()
    k_s = nc.dram_tensor("k_s", [16, 128, T_ALL], BF16).ap()
    v_s = nc.dram_tensor("v_s", [T_ALL, D], BF16).ap()

    x_own = sb("x_own", [128, NCH, T_OWN], F32)
    B1 = sb("B1", [128, NCH, T_ALL], BF16)
    ones_bf = sb("ones_bf", [128, 128], BF16)
    ident_bf = sb("ident_bf", [128, 128], BF16)
    perm_bf = sb("perm_bf", [128, 128], BF16)
    ident_f = sb("ident_f", [128, 128], F32)
    GT = sb("GT", [NE, T_OWN], BF16)
    shared_mod = {"mod": sb("mod", [128, 96, 2], F32), "A1": sb("A1", [128, NCH, 2], F32), "A2": sb("A2", [128, NCH, 2], F32),
                  "gmix": sb("gmix", [128, NCH], F32), "gffn": sb("gffn", [128, NCH], F32)}
    ps = [es.enter_context(nc.psum_tensor("ps%d" % i, [128, 512], F32)) for i in range(8)]

    PS = lambda i: [("ps", i)]

    def dma_in(out_ap, in_ap, writes, cast=False, reads=()):
        q = "pool" if cast else "sp"
        return P.op(q, lambda e: e.dma_start(out=out_ap, in_=in_ap), reads=reads, writes=writes, dma=True)

    def dma_out(out_ap, in_ap, reads, writes=()):
        return P.op("sp", lambda e: e.dma_start(out=out_ap, in_=in_ap), reads=reads, writes=writes, dma=True)

    def mm(out_ap, lhsT, rhs, start, stop, reads, writes):
        return P.op("pe", lambda e: e.matmul(out_ap, lhsT=lhsT, rhs=rhs, start=start, stop=stop),
                    reads=reads, writes=writes)

    def act(out_ap, in_ap, func, reads, writes, bias=None, scale=None):
        kw = {}
        if bias is not None:
            kw["bias"] = bias
        if scale is not None:
            kw["scale"] = scale
        return P.op("act", lambda e: e.activation(out=out_ap, in_=in_ap, func=func, **kw), reads=reads, writes=writes)

    def dve(fn, reads, writes):
        return P.op("dve", fn, reads=reads, writes=writes)

    class Ring:
        def __init__(self, name, n, shape, dt, stack):
            self.t = [sb("%s%d" % (name, i), shape, dt, stack) for i in range(n)]
            self.k = [(name, i) for i in range(n)]
            self.i = 0

        def next(self):
            j = self.i % len(self.t)
            self.i += 1
            return self.t[j], [self.k[j]]

    dma_in(ones_bf[:], cst[:, 0, :], ["ones_bf"], cast=True)
    dma_in(ident_bf[:], cst[:, 1, :], ["ident_bf"], cast=True)
    dma_in(perm_bf[:], cst[:, 2, :], ["perm_bf"], cast=True)
    dma_in(ident_f[:], cst[:, 1, :], ["ident_f"])

    def declare_layer(li):
        kind = KINDS[li]
        nkh = 4 if kind == "gqa" else 16
        nvc = 4 if kind == "gqa" else 16
        nqkv = 16 + nkh + nvc
        s = "_%d" % li
        L = {}
        L["adaw"] = din("adaw" + s, [96, 128, NCH, 128])
        L["adabT"] = din("adabT" + s, [128, 96])
        L["gmixT"] = din("gmixT" + s, [128, NCH])
        L["gffnT"] = din("gffnT" + s, [128, NCH])
        L["wqkv"] = din("wqkv" + s, [nqkv, 128, NCH, 128])
        L["wo"] = din("wo" + s, [NCH, 128, NCH, 128])
        L["rw"] = din("rw" + s, [128, NCH, NE])
        L["rb"] = din("rb" + s, [1, NE])
        L["wg"] = din("wg" + s, [NE, NJ, 128, NCH, 128])
        L["wu"] = din("wu" + s, [NE, NJ, 128, NCH, 128])
        L["bgT"] = din("bgT" + s, [128, NE, NJ])
        L["buT"] = din("buT" + s, [128, NE, NJ])
        L["wd"] = din("wd" + s, [NE, 4, 128, NJ, 512])
        L["bd"] = din("bd" + s, [NE, D])
        if kind == "na":
            L["nabias"] = [din("nabias%s_h%d" % (s, h), [16, 128, NA_NT, 128]) for h in range(2)]
        if kind == "gqa":
            L["qkgT"] = din("qkgT" + s, [128, 2])
        if kind == "diff":
            L["lamT"] = din("lamT" + s, [128, 4, 128])
            L["sublnT"] = din("sublnT" + s, [128, 2])
        L.update(shared_mod)
        return L

    def emit_mods(li, L):
        adaw, adabT, mod, A1, A2, gmix, gffn = (L[k] for k in ("adaw", "adabT", "mod", "A1", "A2", "gmix", "gffn"))
        dma_in(gmix[:], L["gmixT"], ["gmix"])
        dma_in(gffn[:], L["gffnT"], ["gffn"])

        with contextlib.ExitStack() as ph:
            c2 = sb("c2", [128, NCH, 2], F32, ph)
            silu = sb("silu", [128, NCH, 2], BF16, ph)
            adab = sb("adab", [128, 96], F32, ph)
            wr = Ring("wrA", 4, [128, NCH, 128], BF16, ph)
            dma_in(c2[:], c2T, ["c2"])
            dma_in(adab[:], adabT, ["adab"])
            sg0 = sb("sg0", [128, NCH, 2], F32, ph)
            act(sg0[:], c2[:], AF.Sigmoid, ["c2"], ["sg0"])
            dve(lambda e: e.tensor_tensor(out=silu[:], in0=c2[:], in1=sg0[:], op=ALU.mult), ["c2", "sg0"], ["silu"])
            for ob in range(96):
                w, wk = wr.next()
                dma_in(w[:], adaw[ob], wk, cast=True)
                for c in range(NCH):
                    mm(ps[7][:, 2 * ob:2 * ob + 2], w[:, c, :], silu[:, c, :], c == 0, c == NCH - 1,
                       wk + ["silu"], PS(7))
            for v in range(2):
                dve(lambda e, v=v: e.tensor_tensor(out=mod[:, :, v], in0=ps[7][:, v:192:2], in1=adab[:], op=ALU.add),
                    PS(7) + ["adab"], ["mod"])
            for v in range(2):
                dve(lambda e, v=v: e.scalar_tensor_tensor(out=A1[:, :, v], in0=mod[:, 16:32, v], scalar=1.0, in1=gmix[:],
                                                          op0=ALU.add, op1=ALU.mult), ["mod", "gmix"], ["A1"])
                dve(lambda e, v=v: e.scalar_tensor_tensor(out=A2[:, :, v], in0=mod[:, 64:80, v], scalar=1.0, in1=gffn[:],
                                                          op0=ALU.add, op1=ALU.mult), ["mod", "gffn"], ["A2"])
            P.barrier()

    def emit_half(li, hh, L):
        kind = KINDS[li]
        last = li == 3
        nqh = 16
        nkh = 4 if kind == "gqa" else 16
        nvc = 4 if kind == "gqa" else 16
        lam_init = 0.8 - 0.6 * math.exp(-0.3 * li)
        mod, A1, A2 = L["mod"], L["A1"], L["A2"]
        wqkv, wo, rw, rb, wg, wu, bgT, buT, wd, bd = (L[k] for k in ("wqkv", "wo", "rw", "rb", "wg", "wu", "bgT", "buT", "wd", "bd"))
        if kind == "na":
            nabias = L["nabias"][hh]
        if kind in ("gqa", "diff"):
            cosT, sinT = cosT_h[hh], sinT_h[hh]
        if kind == "gqa":
            qkgT = L["qkgT"]
        if kind == "diff":
            lamT, sublnT = L["lamT"], L["sublnT"]
        Xin, xin_id = (x_in, 2) if li == 0 else (Xs[(li - 1) % 2], (li - 1) % 2)
        Xout, xout_id = Xs[li % 2], li % 2
        outT = outT_all[hh]

        def load_x(dst_fn, ls, n, wkeys):
            for (lo, gs, ln) in segs(hh, ls, n):
                dma_in(dst_fn(lo - ls, ln), Xin[:, :, gs:gs + ln], wkeys, reads=xd_keys(xin_id, gs, ln))

        for (oc0, n, mc, ac0) in OWN_TILES:
            load_x(lambda r, ln, oc0=oc0: x_own[:, :, oc0 + r:oc0 + r + ln], ac0, n,
                   [k for c in range(NCH) for k in kcols("X", c, oc0, n)])

        def norm_mod(src, src_keys, dst, dst_keys, n, Amat, shift_seg, mc, tmp_ring, sq_ring, rs_t, bank):
            for c in range(NCH):
                sq, sqk = sq_ring.next()
                act(sq[:, :n], src(c), AF.Square, src_keys(c), sqk)
                mm(ps[bank][:, :n], ones_bf[:], sq[:, :n], c == 0, c == NCH - 1, sqk + ["ones_bf"], PS(bank))
            act(rs_t[:, :n], ps[bank][:, :n], AF.Sqrt, PS(bank), ["rs_t"], bias=EPS, scale=1.0 / D)
            dve(lambda e: e.reciprocal(out=rs_t[:, :n], in_=rs_t[:, :n]), ["rs_t"], ["rs_t"])
            for c in range(NCH):
                tmp, tk = tmp_ring.next()
                sc = src(c)
                dve(lambda e, c=c, tmp=tmp, sc=sc: e.scalar_tensor_tensor(out=tmp[:, :n], in0=sc, scalar=Amat[:, c, mc:mc + 1],
                                                                          in1=rs_t[:, :n], op0=ALU.mult, op1=ALU.mult),
                    src_keys(c) + ["rs_t", "A1", "A2"], tk)
                act(dst(c), tmp[:, :n], AF.Identity, tk + ["mod"], dst_keys(c),
                    bias=mod[:, shift_seg * 16 + c, mc:mc + 1], scale=1.0)

        with contextlib.ExitStack() as ph:
            xr = Ring("xt", 2, [128, NCH, 256], F32, ph)
            tmp_ring = Ring("tmpA", 3, [128, 256], F32, ph)
            sq_ring = Ring("sqA", 3, [128, 256], BF16, ph)
            rs_t = sb("rs_t", [128, 512], F32, ph)
            for (c0, n, mc) in ALL_TILES:
                for s0 in range(c0, c0 + n, 256):
                    xt, xk = xr.next()
                    load_x(lambda r, ln, xt=xt: xt[:, :, r:r + ln], s0, 256, xk)
                    norm_mod(lambda c: xt[:, c, :], lambda c: xk,
                             lambda c: B1[:, c, s0:s0 + 256], lambda c: kcols("B1", c, s0, 256),
                             256, A1, 0, mc, tmp_ring, sq_ring, rs_t, 6)
            P.barrier()

        rope = kind in ("gqa", "diff")
        with contextlib.ExitStack() as ph:
            wr = Ring("wrB", 3, [128, NCH, 128], BF16, ph)
            st_ring = Ring("stB", 4, [128, 512], BF16, ph)
            f_ring = Ring("fB", 6, [128, 512], F32, ph)
            stV = Ring("stV", 2, [128, 18, 128], BF16, ph)
            if rope:
                cos_sb = sb("cos_sb", [128, SEQ], BF16, ph)
                sin_sb = sb("sin_sb", [128, SEQ], BF16, ph)
                dma_in(cos_sb[:], cosT, ["cos_sb"], cast=True)
                dma_in(sin_sb[:], sinT, ["sin_sb"], cast=True)
            if kind == "gqa":
                qkg = sb("qkg", [128, 2], F32, ph)
                dma_in(qkg[:], qkgT, ["qkg"])
            bankc = [0]

            def nb(lo, cnt):
                bankc[0] += 1
                return lo + bankc[0] % cnt

            def do_rope(src_ap, src_keys, n, c0):
                st1, k1 = st_ring.next()
                act(st1[:, :n], src_ap, AF.Identity, src_keys, k1)
                pb = nb(2, 2)
                mm(ps[pb][:, :n], perm_bf[:], st1[:, :n], True, True, k1 + ["perm_bf"], PS(pb))
                f1, fk1 = f_ring.next()
                f2, fk2 = f_ring.next()
                dve(lambda e: e.tensor_tensor(out=f1[:, :n], in0=src_ap, in1=cos_sb[:, c0:c0 + n], op=ALU.mult),
                    src_keys + k1 + ["cos_sb"], fk1)
                dve(lambda e: e.tensor_tensor(out=f2[:, :n], in0=ps[pb][:, :n], in1=sin_sb[:, c0:c0 + n], op=ALU.mult),
                    PS(pb) + ["sin_sb"], fk2)
                st2, k2 = st_ring.next()
                dve(lambda e: e.tensor_tensor(out=st2[:, :n], in0=f1[:, :n], in1=f2[:, :n], op=ALU.add), fk1 + fk2, k2)
                return st2, k2

            def post_qk(pbank, n, is_q, lat, c0):
                src = ps[pbank][:, :n]
                sk = PS(pbank)
                if kind == "na":
                    st, k = st_ring.next()
                    act(st[:, :n], src, AF.Identity, sk, k, scale=(SCALE if is_q else 1.0))
                    return st, k
                if kind == "gqa":
                    sq, sqk = st_ring.next()
                    act(sq[:, :n], src, AF.Square, sk, sqk)
                    pb = nb(2, 2)
                    mm(ps[pb][:, :n], ones_bf[:], sq[:, :n], True, True, sqk + ["ones_bf"], PS(pb))
                    rs, rk = f_ring.next()
                    act(rs[:, :n], ps[pb][:, :n], AF.Sqrt, PS(pb), rk, bias=EPS, scale=1.0 / 128)
                    dve(lambda e: e.reciprocal(out=rs[:, :n], in_=rs[:, :n]), rk, rk)
                    qn, qnk = f_ring.next()
                    gi = 0 if is_q else 1
                    dve(lambda e: e.scalar_tensor_tensor(out=qn[:, :n], in0=src, scalar=qkg[:, gi:gi + 1], in1=rs[:, :n],
                                                         op0=ALU.mult, op1=ALU.mult), sk + rk + ["qkg"], qnk)
                    if lat:
                        return do_rope(qn[:, :n], qnk, n, c0)
                    st, k = st_ring.next()
                    act(st[:, :n], qn[:, :n], AF.Identity, qnk, k)
                    return st, k
                if lat:
                    return do_rope(src, sk, n, c0)
                st, k = st_ring.next()
                act(st[:, :n], src, AF.Identity, sk, k)
                return st, k

            for hq in range(nqh):
                w, wk = wr.next()
                dma_in(w[:], wqkv[hq], wk, cast=True)
                for (oc0, n, mc, ac0) in OWN_TILES:
                    if last and mc == 1:
                        continue
                    pbk = nb(0, 2)
                    for c in range(NCH):
                        mm(ps[pbk][:, :n], w[:, c, :], B1[:, c, ac0:ac0 + n], c == 0, c == NCH - 1,
                           wk + kcols("B1", c, ac0, n), PS(pbk))
                    st, k = post_qk(pbk, n, True, mc == 0, ac0)
                    dma_out(q_s[hq][:, oc0:oc0 + n], st[:, :n], k, [("q_s", hq)])
            for hk in range(nkh):
                w, wk = wr.next()
                dma_in(w[:], wqkv[nqh + hk], wk, cast=True)
                for (c0, n, mc) in ALL_TILES:
                    pbk = nb(0, 2)
                    for c in range(NCH):
                        mm(ps[pbk][:, :n], w[:, c, :], B1[:, c, c0:c0 + n], c == 0, c == NCH - 1,
                           wk + kcols("B1", c, c0, n), PS(pbk))
                    st, k = post_qk(pbk, n, False, mc == 0, c0)
                    dma_out(k_s[hk][:, c0:c0 + n], st[:, :n], k, [("k_s", hk)])
            v_view = v_s.rearrange("(kc p) n -> p kc n", p=128)
            for vc in range(nvc):
                w, wk = wr.next()
                dma_in(w[:], wqkv[nqh + nkh + vc], wk, cast=True)
                sv, svk = stV.next()
                for g0 in range(0, 18, 4):
                    gn = min(4, 18 - g0)
                    pbk = nb(4, 2)
                    for gi in range(gn):
                        kc = g0 + gi
                        for c in range(NCH):
                            mm(ps[pbk][:, gi * 128:(gi + 1) * 128], B1[:, c, kc * 128:(kc + 1) * 128], w[:, c, :],
                               c == 0, c == NCH - 1, wk + kcols("B1", c, kc * 128, 128), PS(pbk))
                    act(sv[:, g0:g0 + gn, :], ps[pbk][:, :gn * 128].rearrange("p (a b) -> p a b", b=128), AF.Identity,
                        PS(pbk), svk)
                dma_out(v_view[:, :, vc * 128:(vc + 1) * 128], sv[:], svk, [("v_s", vc)])
            P.barrier()

        with contextlib.ExitStack() as ph:
            dv = 256 if kind == "diff" else 128
            q_ring = Ring("qh", 2, [128, T_OWN], BF16, ph)
            k_ring = Ring("kh", 2, [128, T_ALL], BF16, ph)
            v_ring = Ring("vh", 2, [128, 18, dv], BF16, ph)
            e_ring = Ring("E", 3, [128, 512], BF16, ph)
            f_ring = Ring("fC", 8 if kind == "diff" else 3, [128, 512], F32, ph)
            if kind == "na":
                b_ring = Ring("nab", 2, [128, NA_NT, 128], BF16, ph)
            if kind == "diff":
                lam = sb("lam", [128, 4, 128], F32, ph)
                subln = sb("subln", [128, 2], F32, ph)
                lamw = sb("lamw", [128, 4], F32, ph)
                lamp = sb("lamp", [128, 2, 128], F32, ph)
                dma_in(lam[:], lamT, ["lam"])
                dma_in(subln[:], sublnT, ["subln"])
                dve(lambda e: e.tensor_tensor(out=lamp[:, 0, :], in0=lam[:, 0, :], in1=lam[:, 1, :], op=ALU.mult), ["lam"], ["lamp"])
                dve(lambda e: e.tensor_tensor(out=lamp[:, 1, :], in0=lam[:, 2, :], in1=lam[:, 3, :], op=ALU.mult), ["lam"], ["lamp"])
                dve(lambda e: e.reduce_sum(out=lamw[:, 0:1], in_=lamp[:, 0, :], axis=AX.X), ["lamp"], ["lamw"])
                dve(lambda e: e.reduce_sum(out=lamw[:, 1:2], in_=lamp[:, 1, :], axis=AX.X), ["lamp"], ["lamw"])
                act(lamw[:, 2:4], lamw[:, 0:2], AF.Exp, ["lamw"], ["lamw"])
                dve(lambda e: e.scalar_tensor_tensor(out=lamw[:, 0:1], in0=lamw[:, 3:4], scalar=-lam_init, in1=lamw[:, 2:3],
                                                     op0=ALU.add, op1=ALU.subtract), ["lamw"], ["lamw"])
                dve(lambda e: e.tensor_scalar(out=subln[:], in0=subln[:], scalar1=1.0 - lam_init, scalar2=None, op0=ALU.mult),
                    ["subln"], ["subln"])
            cnt = {"s": 0, "o": 0, "d": 0}

            def bank(kind_, lo, n_):
                cnt[kind_] += 1
                return lo + cnt[kind_] % n_

            def attend(qt, qk, kt, kk, vt, vk, q0, n, chunks, ndv, bias=None, bk=None):
                pO = [bank("o", 2, 4) for _ in range(ndv)]
                pD = bank("d", 6, 2)
                nchk = len(chunks)
                for i, (kc, tid) in enumerate(chunks):
                    sbk = bank("s", 0, 2)
                    mm(ps[sbk][:, :n], kt[:, kc * 128:(kc + 1) * 128], qt[:, q0:q0 + n], True, tid is None, kk + qk, PS(sbk))
                    if tid is not None:
                        mm(ps[sbk][:, :n], ident_bf[:], bias[:, tid, :], False, True, bk + ["ident_bf"], PS(sbk))
                    E, ek = e_ring.next()
                    act(E[:, :n], ps[sbk][:, :n], AF.Exp, PS(sbk), ek, scale=(1.0 if kind == "na" else SCALE))
                    for dvc in range(ndv):
                        mm(ps[pO[dvc]][:, :n], vt[:, kc, dvc * 128:(dvc + 1) * 128], E[:, :n], i == 0, i == nchk - 1,
                           vk + ek, PS(pO[dvc]))
                    mm(ps[pD][:, :n], ones_bf[:], E[:, :n], i == 0, i == nchk - 1, ek + ["ones_bf"], PS(pD))
                rd, rk = f_ring.next()
                dve(lambda e: e.reciprocal(out=rd[:, :n], in_=ps[pD][:, :n]), PS(pD), rk)
                return pO, rd, rk

            def q_tiles():
                ctxq = [] if last else [(1024, 128, [(16, None), (17, None)])]
                if kind == "na":
                    return [(pl * 128, 128, [(16, None), (17, None)] + [(kc, tid) for (kc, _, tid) in NA_PLAN[pl]])
                            for pl in range(8)] + ctxq
                allc = [(kc, None) for kc in range(18)]
                return [(0, 512, allc), (512, 512, allc)] + ctxq

            def load_head(hq):
                qt, qk = q_ring.next()
                kt, kk = k_ring.next()
                dma_in(qt[:], q_s[hq], qk, reads=[("q_s", hq)])
                hk = hq // 4 if kind == "gqa" else hq
                dma_in(kt[:], k_s[hk], kk, reads=[("k_s", hk)])
                return qt, qk, kt, kk

            if kind != "diff":
                for hq in range(16):
                    qt, qk, kt, kk = load_head(hq)
                    vt, vk = v_ring.next()
                    vcol = (hq // 4 if kind == "gqa" else hq) * 128
                    dma_in(vt[:], v_view[:, :, vcol:vcol + 128], vk, reads=[("v_s", vcol // 128)])
                    bias = bk = None
                    if kind == "na":
                        bias, bk = b_ring.next()
                        dma_in(bias[:], nabias[hq], bk, cast=True)
                    for (q0, n, chunks) in q_tiles():
                        pO, rd, rk = attend(qt, qk, kt, kk, vt, vk, q0, n, chunks, 1, bias, bk)
                        dve(lambda e, pO=pO, rd=rd, q0=q0, n=n, hq=hq: e.tensor_tensor(
                            out=B1[:, hq, q0:q0 + n], in0=ps[pO[0]][:, :n], in1=rd[:, :n], op=ALU.mult),
                            PS(pO[0]) + rk, kcols("B1", hq, q0, n))
            else:
                for hd in range(8):
                    qa = load_head(2 * hd)
                    vt, vk = v_ring.next()
                    dma_in(vt[:], v_view[:, :, hd * 256:(hd + 1) * 256], vk, reads=[("v_s", 2 * hd), ("v_s", 2 * hd + 1)])
                    qb = load_head(2 * hd + 1)
                    for (q0, n, chunks) in q_tiles():
                        pO, rd, rk = attend(qa[0], qa[1], qa[2], qa[3], vt, vk, q0, n, chunks, 2)
                        o0 = []
                        for dvc in range(2):
                            f, fk = f_ring.next()
                            dve(lambda e, f=f, p=pO[dvc], rd=rd, n=n: e.tensor_tensor(out=f[:, :n], in0=ps[p][:, :n], in1=rd[:, :n],
                                                                                     op=ALU.mult), PS(pO[dvc]) + rk, fk)
                            o0.append((f, fk))
                        pO, rd, rk = attend(qb[0], qb[1], qb[2], qb[3], vt, vk, q0, n, chunks, 2)
                        od = []
                        sbk = bank("s", 0, 2)
                        for dvc in range(2):
                            f, fk = f_ring.next()
                            dve(lambda e, f=f, p=pO[dvc], rd=rd, n=n: e.tensor_tensor(out=f[:, :n], in0=ps[p][:, :n], in1=rd[:, :n],
                                                                                     op=ALU.mult), PS(pO[dvc]) + rk, fk)
                            dve(lambda e, f=f, o=o0[dvc][0], n=n: e.scalar_tensor_tensor(out=f[:, :n], in0=f[:, :n], scalar=lamw[:, 0:1],
                                                                                          in1=o[:, :n], op0=ALU.mult, op1=ALU.add),
                                fk + o0[dvc][1] + ["lamw"], fk)
                            od.append((f, fk))
                            E, ek = e_ring.next()
                            act(E[:, :n], f[:, :n], AF.Square, fk, ek)
                            mm(ps[sbk][:, :n], ones_bf[:], E[:, :n], dvc == 0, dvc == 1, ek + ["ones_bf"], PS(sbk))
                        rs, rsk = f_ring.next()
                        act(rs[:, :n], ps[sbk][:, :n], AF.Sqrt, PS(sbk), rsk, bias=EPS, scale=1.0 / 256)
                        dve(lambda e, rs=rs, n=n: e.reciprocal(out=rs[:, :n], in_=rs[:, :n]), rsk, rsk)
                        for dvc in range(2):
                            hidx = 2 * hd + dvc
                            dve(lambda e, f=od[dvc][0], rs=rs, n=n, q0=q0, hidx=hidx, dvc=dvc: e.scalar_tensor_tensor(
                                out=B1[:, hidx, q0:q0 + n], in0=f[:, :n], scalar=subln[:, dvc:dvc + 1], in1=rs[:, :n],
                                op0=ALU.mult, op1=ALU.mult), od[dvc][1] + rsk + ["subln"], kcols("B1", hidx, q0, n))
            P.barrier()

        own_tiles = [t for t in OWN_TILES if not (last and t[2] == 1)]
        HO = T_OWN
        with contextlib.ExitStack() as ph:
            wr = Ring("wrD", 3, [128, NCH, 128], BF16, ph)
            tmp_ring = Ring("tmpD", 3, [128, 256], F32, ph)
            sq_ring = Ring("sqD", 3, [128, 256], BF16, ph)
            rs_t2 = sb("rs_t", [128, 512], F32, ph)
            rw_bf = sb("rw_bf", [128, NCH, NE], BF16, ph)
            rb_bf = sb("rb_bf", [1, NE], BF16, ph)
            r_ring = Ring("rt", 2, [128, 6, NE], F32, ph)
            dma_in(rw_bf[:], rw, ["rw_bf"], cast=True)
            dma_in(rb_bf[:], rb, ["rb_bf"], cast=True)
            bc = 0
            for fc in range(NCH):
                w, wk = wr.next()
                dma_in(w[:], wo[fc], wk, cast=True)
                for (oc0, n, mc, ac0) in own_tiles:
                    bc += 1
                    b = bc % 2
                    for h in range(NCH):
                        mm(ps[b][:, :n], w[:, h, :], B1[:, h, oc0:oc0 + n], h == 0, h == NCH - 1,
                           wk + kcols("B1", h, oc0, n), PS(b))
                    xk = kcols("X", fc, oc0, n)
                    dve(lambda e, b=b, n=n, fc=fc, oc0=oc0, mc=mc: e.scalar_tensor_tensor(
                        out=x_own[:, fc, oc0:oc0 + n], in0=ps[b][:, :n], scalar=mod[:, 32 + fc, mc:mc + 1],
                        in1=x_own[:, fc, oc0:oc0 + n], op0=ALU.mult, op1=ALU.add), PS(b) + xk + ["mod"], xk)
            for (oc0, n, mc, ac0) in own_tiles:
                for s0 in range(oc0, oc0 + n, 256):
                    nn = min(256, oc0 + n - s0)
                    norm_mod(lambda c: x_own[:, c, s0:s0 + nn], lambda c: kcols("X", c, s0, nn),
                             lambda c: B1[:, c, HO + s0:HO + s0 + nn], lambda c: kcols("B1", c, HO + s0, nn),
                             nn, A2, 3, mc, tmp_ring, sq_ring, rs_t2, 6)
            nblk = sum(t[1] for t in own_tiles) // 128
            for blk in range(nblk):
                b = 2 + blk % 2
                c0 = HO + blk * 128
                for c in range(NCH):
                    mm(ps[b][:, 0:NE], B1[:, c, c0:c0 + 128], rw_bf[:, c, :], c == 0, False,
                       kcols("B1", c, c0, 128) + ["rw_bf"], PS(b))
                mm(ps[b][:, 0:NE], ones_bf[0:1, :], rb_bf[0:1, :], False, True, ["ones_bf", "rb_bf"], PS(b))
                rt, rk = r_ring.next()
                lg, t8, mk, ex, sm, G = (rt[:, 0, :], rt[:, 1, :], rt[:, 2, :], rt[:, 3, :], rt[:, 4, :], rt[:, 5, :])
                dve(lambda e, lg=lg, b=b: e.tensor_copy(out=lg, in_=ps[b][:, 0:NE]), PS(b), rk)
                dve(lambda e, lg=lg, t8=t8: e.max(out=t8[:, 0:8], in_=lg), rk, rk)
                dve(lambda e, lg=lg, t8=t8, mk=mk: e.tensor_scalar(out=mk, in0=lg, scalar1=t8[:, 3:4], scalar2=None, op0=ALU.is_ge),
                    rk, rk)
                dve(lambda e, t8=t8, sm=sm: e.tensor_scalar(out=sm[:, 0:1], in0=t8[:, 0:1], scalar1=-1.0, scalar2=None, op0=ALU.mult),
                    rk, rk)
                act(ex, lg, AF.Exp, rk, rk, bias=sm[:, 0:1], scale=1.0)
                dve(lambda e, ex=ex, mk=mk: e.tensor_tensor(out=ex, in0=ex, in1=mk, op=ALU.mult), rk, rk)
                dve(lambda e, ex=ex, sm=sm: e.reduce_sum(out=sm[:, 1:2], in_=ex, axis=AX.X), rk, rk)
                dve(lambda e, sm=sm: e.reciprocal(out=sm[:, 2:3], in_=sm[:, 1:2]), rk, rk)
                dve(lambda e, ex=ex, sm=sm, G=G: e.tensor_scalar(out=G, in0=ex, scalar1=sm[:, 2:3], scalar2=None, op0=ALU.mult), rk, rk)
                b2 = 4 + blk % 2
                P.op("pe", lambda e, G=G, b2=b2: e.transpose(out=ps[b2][0:NE, 0:128], in_=G, identity=ident_f[:]),
                     reads=rk + ["ident_f"], writes=PS(b2))
                act(GT[0:NE, blk * 128:(blk + 1) * 128], ps[b2][0:NE, 0:128], AF.Identity, PS(b2), [("GT", blk)])
            P.barrier()

        with contextlib.ExitStack() as ph:
            wr = Ring("wrE", 3, [128, NCH, 128], BF16, ph)
            wdr = Ring("wdE", 2, [128, NJ, 512], BF16, ph)
            actG = sb("actG", [128, NJ, T_OWN], BF16, ph)
            gte_r = Ring("GTe", 1, [NE, T_OWN], BF16, ph)
            gb_r = Ring("Gb", 1, [128, T_OWN], BF16, ph)
            gf_r = Ring("gf", 2, [128, 512], F32, ph)
            sg_r = Ring("sgE", 2, [128, 512], BF16, ph)
            uf_r = Ring("uf", 2, [128, 512], F32, ph)
            bg = sb("bg", [128, NE, NJ], F32, ph)
            bu = sb("bu", [128, NE, NJ], F32, ph)
            bd_bf = sb("bd_bf", [NE, D], BF16, ph)
            dma_in(bg[:], bgT, ["bg"])
            dma_in(bu[:], buT, ["bu"])
            dma_in(bd_bf[:], bd, ["bd_bf"], cast=True)
            GTk = [("GT", blk) for blk in range(9)]
            ncols = sum(t[1] for t in own_tiles)
            pc = 0
            for fc in range(NCH):
                for (oc0, n, mc, ac0) in own_tiles:
                    pc += 1
                    b = pc % 2
                    mm(ps[b][:, :n], bd_bf[0:NE, fc * 128:(fc + 1) * 128], GT[0:NE, oc0:oc0 + n], True, True,
                       ["bd_bf"] + GTk, PS(b))
                    xk = kcols("X", fc, oc0, n)
                    dve(lambda e, b=b, n=n, fc=fc, oc0=oc0, mc=mc: e.scalar_tensor_tensor(
                        out=x_own[:, fc, oc0:oc0 + n], in0=ps[b][:, :n], scalar=mod[:, 80 + fc, mc:mc + 1],
                        in1=x_own[:, fc, oc0:oc0 + n], op0=ALU.mult, op1=ALU.add), PS(b) + xk + ["mod"], xk)
            for ex in range(NE):
                gte, gtek = gte_r.next()
                dve(lambda e, gte=gte, ex=ex: e.tensor_scalar(out=gte[0:NE, 0:ncols], in0=GT[0:NE, 0:ncols],
                                                              scalar1=ident_f[0:NE, ex:ex + 1], scalar2=None, op0=ALU.mult),
                    GTk + ["ident_f"], gtek)
                gb, gbk = gb_r.next()
                for (oc0, n, mc, ac0) in own_tiles:
                    pc += 1
                    b = pc % 2
                    mm(ps[b][:, :n], ones_bf[0:NE, :], gte[0:NE, oc0:oc0 + n], True, True, gtek + ["ones_bf"], PS(b))
                    act(gb[:, oc0:oc0 + n], ps[b][:, :n], AF.Identity, PS(b), gbk)
                for j in range(NJ):
                    wgb, wgk = wr.next()
                    dma_in(wgb[:], wg[ex, j], wgk, cast=True)
                    wub, wuk = wr.next()
                    dma_in(wub[:], wu[ex, j], wuk, cast=True)
                    for (oc0, n, mc, ac0) in own_tiles:
                        pc += 1
                        pg = 2 * (pc % 2)
                        pu = pg + 1
                        for c in range(NCH):
                            mm(ps[pg][:, :n], wgb[:, c, :], B1[:, c, HO + oc0:HO + oc0 + n], c == 0, c == NCH - 1,
                               wgk + kcols("B1", c, HO + oc0, n), PS(pg))
                        for c in range(NCH):
                            mm(ps[pu][:, :n], wub[:, c, :], B1[:, c, HO + oc0:HO + oc0 + n], c == 0, c == NCH - 1,
                               wuk + kcols("B1", c, HO + oc0, n), PS(pu))
                        gf, gfk = gf_r.next()
                        sg, sgk = sg_r.next()
                        uf, ufk = uf_r.next()
                        dve(lambda e, gf=gf, pg=pg, n=n, ex=ex, j=j: e.tensor_scalar(
                            out=gf[:, :n], in0=ps[pg][:, :n], scalar1=bg[:, ex, j:j + 1], scalar2=7.0, op0=ALU.add, op1=ALU.min),
                            PS(pg) + ["bg"], gfk)
                        act(sg[:, :n], gf[:, :n], AF.Sigmoid, gfk, sgk, scale=1.702)
                        dve(lambda e, uf=uf, pu=pu, n=n, ex=ex, j=j: e.tensor_scalar(
                            out=uf[:, :n], in0=ps[pu][:, :n], scalar1=bu[:, ex, j:j + 1], scalar2=7.0, op0=ALU.add, op1=ALU.min),
                            PS(pu) + ["bu"], ufk)
                        dve(lambda e, uf=uf, n=n: e.tensor_scalar(out=uf[:, :n], in0=uf[:, :n], scalar1=-7.0, scalar2=1.0,
                                                                  op0=ALU.max, op1=ALU.add), ufk, ufk)
                        dve(lambda e, gf=gf, sg=sg, n=n: e.tensor_tensor(out=gf[:, :n], in0=gf[:, :n], in1=sg[:, :n], op=ALU.mult),
                            gfk + sgk, gfk)
                        dve(lambda e, gf=gf, uf=uf, n=n: e.tensor_tensor(out=gf[:, :n], in0=gf[:, :n], in1=uf[:, :n], op=ALU.mult),
                            gfk + ufk, gfk)
                        ak = kcols("AG", j, oc0, n)
                        dve(lambda e, gf=gf, gb=gb, n=n, j=j, oc0=oc0: e.tensor_tensor(
                            out=actG[:, j, oc0:oc0 + n], in0=gf[:, :n], in1=gb[:, oc0:oc0 + n], op=ALU.mult), gfk + gbk, ak)
                for grp in range(4):
                    wdb, wdk = wdr.next()
                    dma_in(wdb[:], wd[ex, grp], wdk, cast=True)
                    for (oc0, n, mc, ac0) in own_tiles:
                        for f4 in range(4):
                            fc = grp * 4 + f4
                            pc += 1
                            b = 4 + pc % 4
                            for j in range(NJ):
                                mm(ps[b][:, :n], wdb[:, j, f4 * 128:(f4 + 1) * 128], actG[:, j, oc0:oc0 + n], j == 0, j == NJ - 1,
                                   wdk + kcols("AG", j, oc0, n), PS(b))
                            xk = kcols("X", fc, oc0, n)
                            dve(lambda e, b=b, n=n, fc=fc, oc0=oc0, mc=mc: e.scalar_tensor_tensor(
                                out=x_own[:, fc, oc0:oc0 + n], in0=ps[b][:, :n], scalar=mod[:, 80 + fc, mc:mc + 1],
                                in1=x_own[:, fc, oc0:oc0 + n], op0=ALU.mult, op1=ALU.add), PS(b) + xk + ["mod"], xk)
            P.barrier()

        if not last:
            for (oc0, n, mc, ac0) in OWN_TILES:
                for (lo, gs, ln) in segs(hh, ac0, n):
                    dma_out(Xout[:, :, gs:gs + ln], x_own[:, :, oc0 + lo - ac0:oc0 + lo - ac0 + ln],
                            [k for c in range(NCH) for k in kcols("X", c, oc0, n)], xd_keys(xout_id, gs, ln))
        else:
            with contextlib.ExitStack() as ph:
                fg = sb("fg", [128, NCH], F32, ph)
                dma_in(fg[:], fgT, ["fg"])
                sq_ring = Ring("sqF", 3, [128, 256], BF16, ph)
                o_ring = Ring("oF", 2, [128, NCH, 256], F32, ph)
                rs_f = sb("rs_f", [128, 256], F32, ph)
                for s0 in range(0, 1024, 256):
                    for c in range(NCH):
                        sq, sqk = sq_ring.next()
                        act(sq[:], x_own[:, c, s0:s0 + 256], AF.Square, kcols("X", c, s0, 256), sqk)
                        mm(ps[0][:, :256], ones_bf[:], sq[:], c == 0, c == NCH - 1, sqk + ["ones_bf"], PS(0))
                    act(rs_f[:], ps[0][:, :256], AF.Sqrt, PS(0), ["rs_f"], bias=EPS, scale=1.0 / D)
                    dve(lambda e: e.reciprocal(out=rs_f[:], in_=rs_f[:]), ["rs_f"], ["rs_f"])
                    ot, ok_ = o_ring.next()
                    for c in range(NCH):
                        dve(lambda e, c=c, ot=ot, s0=s0: e.scalar_tensor_tensor(
                            out=ot[:, c, :], in0=x_own[:, c, s0:s0 + 256], scalar=fg[:, c:c + 1], in1=rs_f[:],
                            op0=ALU.mult, op1=ALU.mult), kcols("X", c, s0, 256) + ["rs_f", "fg"], ok_)
                    dma_out(outT[:, :, s0:s0 + 256], ot[:], ok_)


    for li in range(4):
        L = declare_layer(li)
        emit_mods(li, L)
        for hh in range(2):
            emit_half(li, hh, L)


_PROG_CACHE = {}


def get_prog():
    if "f" not in _PROG_CACHE:
        _PROG_CACHE["f"] = build_and_emit(fused_body)
    return _PROG_CACHE["f"]


def _blk(W):
    K, N = W.shape
    return np.ascontiguousarray(W.reshape(K // 128, 128, N // 128, 128).transpose(2, 1, 0, 3))


def _fm(v, n):
    return np.ascontiguousarray(v.reshape(n, 128).T)


def _consts():
    cst = np.zeros((128, 4, 128), np.float32)
    cst[:, 0, :] = 1.0
    cst[:, 1, :] = np.eye(128, dtype=np.float32)
    idx = np.arange(128)
    cst[idx ^ 32, 2, idx] = 1.0
    return cst


def _rope_tables(h):
    t = (np.arange(SEQ) + 1024 * h) % SEQ
    row = (t // 64).astype(np.float32)
    col = (t % 64).astype(np.float32)
    half = 64
    inv_freq = (np.float32(10000.0) ** (-np.arange(0, half, 2, dtype=np.float32) / np.float32(half))).astype(np.float32)
    ang_r = row[:, None] * inv_freq[None, :]
    ang_c = col[:, None] * inv_freq[None, :]
    ang = np.concatenate([ang_r, ang_r, ang_c, ang_c], axis=-1)
    cos = np.cos(ang).astype(np.float32)
    sin = np.sin(ang).astype(np.float32)
    sgn = np.ones(128, np.float32)
    sgn[0:32] = -1.0
    sgn[64:96] = -1.0
    return np.ascontiguousarray(cos.T), np.ascontiguousarray((sin * sgn[None, :]).T)


def _na_bias(rpb, h):
    out = np.full((16, 128, NA_NT, 128), -30000.0, np.float32)
    ki = np.arange(128)[:, None]
    qi = np.arange(128)[None, :]
    for pl in range(8):
        for (kcm, cl, tid) in NA_PLAN[pl]:
            kr = 2 * cl + ki // 64
            kc_ = ki % 64
            ql = 2 * pl + qi // 64
            qc = qi % 64
            r = 16 * h + ql
            rp = 16 * h + kr
            r0 = np.clip(r - 4, 0, 24)
            cs = np.clip(qc - 8, 0, 48)
            valid = (rp >= 0) & (rp <= 31) & (rp >= r0) & (rp < r0 + 8) & (kc_ >= cs) & (kc_ < cs + 16)
            dri = np.clip(rp - r + 7, 0, 14)
            dci = np.clip(kc_ - qc + 15, 0, 30)
            vals = rpb[:, dri, dci]
            out[:, :, tid, :] = np.where(valid[None], vals, np.float32(-30000.0))
    return out


def _layer_inputs(inp, li):
    kind = KINDS[li]
    j = li // 3
    sfx = "_%d" % li
    m = {}
    m["adaw"] = _blk(inp["ada_w"][li])
    m["adabT"] = _fm(inp["ada_b"][li], 96)
    m["gmixT"] = _fm(inp["norm_mix_g"][li], 16)
    m["gffnT"] = _fm(inp["norm_ffn_g"][li], 16)
    if kind == "na":
        m["wqkv"] = _blk(inp["na_wqkv"][j])
        m["wo"] = _blk(inp["na_wo"][j])
    elif kind == "gqa":
        m["wqkv"] = _blk(inp["gqa_wqkv"][j])
        m["wo"] = _blk(inp["gqa_wo"][j])
        m["qkgT"] = np.ascontiguousarray(np.stack([inp["gqa_q_g"][j], inp["gqa_k_g"][j]], axis=1))
    else:
        m["wqkv"] = _blk(inp["diff_wqkv"][j])
        m["wo"] = _blk(inp["diff_wo"][j])
        m["lamT"] = np.ascontiguousarray(np.broadcast_to(inp["diff_lambda"][j][None], (128, 4, 128)))
        m["sublnT"] = _fm(inp["diff_subln_g"][j], 2)
    m["rw"] = np.ascontiguousarray(inp["router_w"][li].reshape(16, 128, NE).transpose(1, 0, 2))
    m["rb"] = np.ascontiguousarray(inp["router_b"][li].reshape(1, NE))
    m["wg"] = np.ascontiguousarray(inp["w_gate"][li].reshape(NE, 16, 128, NJ, 128).transpose(0, 3, 2, 1, 4))
    m["wu"] = np.ascontiguousarray(inp["w_up"][li].reshape(NE, 16, 128, NJ, 128).transpose(0, 3, 2, 1, 4))
    m["bgT"] = np.ascontiguousarray(inp["b_gate"][li].reshape(NE, NJ, 128).transpose(2, 0, 1))
    m["buT"] = np.ascontiguousarray(inp["b_up"][li].reshape(NE, NJ, 128).transpose(2, 0, 1))
    m["wd"] = np.ascontiguousarray(inp["w_down"][li].reshape(NE, NJ, 128, 4, 512).transpose(0, 3, 2, 1, 4))
    m["bd"] = np.ascontiguousarray(inp["b_down"][li])
    out = {k + sfx: v for k, v in m.items()}
    if kind == "na":
        for h in range(2):
            out["nabias%s_h%d" % (sfx, h)] = _na_bias(inp["na_rpb"][j], h)
    return out


def _to_fm(a):
    T = a.shape[0]
    return np.ascontiguousarray(a.T.reshape(16, 128, T).transpose(1, 0, 2))


def kernel(**inp):
    inp = {k: np.asarray(v) for k, v in inp.items()}
    nc = get_prog()
    shared = {"cst": _consts(), "fgT": _fm(inp["final_g"], 16)}
    for h in range(2):
        shared["cosT_h%d" % h], shared["sinT_h%d" % h] = _rope_tables(h)
    for li in range(4):
        shared.update(_layer_inputs(inp, li))
    in_maps = []
    per_b = {}
    for core in range(8):
        b = core // 2
        if b not in per_b:
            c2 = np.stack([inp["c"][b], inp["c_ctx"]], axis=0)
            per_b[b] = {
                "x_in": np.concatenate([_to_fm(inp["x"][b]), _to_fm(inp["ctx"][b])], axis=2),
                "c2T": np.ascontiguousarray(c2.reshape(2, 16, 128).transpose(2, 1, 0)),
            }
        m = dict(shared)
        m.update(per_b[b])
        in_maps.append(m)
    res = run_bass_kernel_spmd(nc, in_maps, core_ids=list(range(8)))
    outs = res.results
    out = np.empty((4, SEQ, D), np.float32)
    for b in range(4):
        o = outs[2 * b]["outT"]
        for h in range(2):
            out[b, 1024 * h:1024 * (h + 1), :] = o[h].transpose(2, 1, 0).reshape(1024, D)
    return out
```

```python
import math
import contextlib
import numpy as np
import ml_dtypes
import concourse.bass as bass
import concourse.mybir as mybir
from concourse.bass_utils import run_bass_kernel_spmd

F32 = mybir.dt.float32
BF16 = mybir.dt.bfloat16
AF = mybir.ActivationFunctionType
ALU = mybir.AluOpType
AX = mybir.AxisListType

ENGS = ("pe", "act", "dve", "pool", "sp")
DMA_SLOTS = 6


class Op:
    __slots__ = ("eng", "fn", "deps", "is_dma", "needs_inc", "token", "idx")

    def __init__(self, eng, fn, is_dma):
        self.eng = eng
        self.fn = fn
        self.deps = []
        self.is_dma = is_dma
        self.needs_inc = is_dma
        self.token = None
        self.idx = -1


class Prog:
    def __init__(self, nc):
        self.nc = nc
        self.eng_ops = {e: [] for e in ENGS}
        self.last_w = {}
        self.readers = {}
        self.dma_count = {e: 0 for e in ENGS}
        self.dma_hist = {e: [] for e in ENGS}
        self.final_wait = []

    def op(self, eng, fn, reads=(), writes=(), dma=False):
        o = Op(eng, fn, dma)
        deps = set()
        for k in reads:
            w = self.last_w.get(k)
            if w is not None:
                deps.add(w)
        for k in writes:
            w = self.last_w.get(k)
            if w is not None:
                deps.add(w)
            for r in self.readers.get(k, ()):
                deps.add(r)
        if dma:
            hist = self.dma_hist[eng]
            if len(hist) >= DMA_SLOTS:
                deps.add(hist[-DMA_SLOTS])
            hist.append(o)
        deps.discard(o)
        for d in deps:
            if d.eng == "pe" and eng == "pe" and not d.is_dma and not dma:
                continue
            o.deps.append(d)
            d.needs_inc = True
        for k in writes:
            self.last_w[k] = o
            self.readers[k] = []
        for k in reads:
            if k in writes:
                continue
            lst = self.readers.setdefault(k, [])
            if not dma:
                lst[:] = [r for r in lst if r.is_dma or r.eng != eng]
            lst.append(o)
        o.idx = len(self.eng_ops[eng])
        self.eng_ops[eng].append(o)
        return o

    def barrier(self):
        lasts = []
        for e in ENGS:
            for o in reversed(self.eng_ops[e]):
                if not o.is_dma and o.fn is not None:
                    lasts.append(o)
                    break
            lasts.extend(self.dma_hist[e][-DMA_SLOTS:])
        for e in ENGS:
            o = Op(e, None, False)
            for d in lasts:
                o.deps.append(d)
                d.needs_inc = True
            self.eng_ops[e].append(o)

    def emit(self, sems, dsems):
        nc = self.nc
        for e in ENGS:
            cnt = 0
            dcnt = 0
            for o in self.eng_ops[e]:
                if o.is_dma:
                    slot = dcnt % DMA_SLOTS
                    o.token = (dsems[e][slot], 16 * (dcnt // DMA_SLOTS + 1))
                    dcnt += 1
                elif o.needs_inc:
                    cnt += 1
                    o.token = (sems[e], cnt)
        all_dma = [o for e in ENGS for o in self.eng_ops[e] if o.is_dma]
        handles = {"pe": nc.tensor, "act": nc.scalar, "dve": nc.vector, "pool": nc.gpsimd, "sp": nc.sync}

        def run_engine(e, eng):
            known = {}
            for o in self.eng_ops[e]:
                waits = {}
                for d in o.deps:
                    s, v = d.token
                    key = id(s)
                    if key not in waits or waits[key][1] < v:
                        waits[key] = (s, v)
                for key, (s, v) in waits.items():
                    if known.get(key, 0) >= v:
                        continue
                    eng.wait_ge(s, v)
                    known[key] = v
                if o.fn is None:
                    continue
                ins = o.fn(eng)
                if o.token is not None:
                    s, v = o.token
                    ins.then_inc(s, 16 if o.is_dma else 1)
            if e == "sp":
                last = {}
                for o in all_dma:
                    s, v = o.token
                    key = id(s)
                    if key not in last or last[key][1] < v:
                        last[key] = (s, v)
                for key, (s, v) in last.items():
                    if known.get(key, 0) < v:
                        eng.wait_ge(s, v)

        with nc.Block() as block:
            @block.tensor
            def _(eng):
                run_engine("pe", eng)

            @block.scalar
            def _(eng):
                run_engine("act", eng)

            @block.vector
            def _(eng):
                run_engine("dve", eng)

            @block.gpsimd
            def _(eng):
                run_engine("pool", eng)

            @block.sync
            def _(eng):
                run_engine("sp", eng)


def build_and_emit(body):
    nc = bass.Bass("TRN2", target_bir_lowering=False)
    with contextlib.ExitStack() as es:
        P = Prog(nc)
        body(nc, P, es)
        sems = {e: es.enter_context(nc.semaphore("s_" + e)) for e in ENGS}
        dsems = {e: [es.enter_context(nc.semaphore("d_%s%d" % (e, i))) for i in range(DMA_SLOTS)]
                 for e in ("sp", "pool", "act")}
        dsems["pe"] = dsems["dve"] = None
        P.emit(sems, dsems)
    return nc


D = 2048
NCH = 16
LCTX = 256
SEQ = 2048
T_ALL = 2304
T_OWN = 1152
NE = 32
DFF = 768
NJ = 6
EPS = 1e-6
SCALE = 128 ** -0.5
KINDS = ("na", "gqa", "diff", "na")
ALL_TILES = [(0, 512, 0), (512, 512, 0), (1024, 512, 0), (1536, 512, 0), (2048, 256, 1)]
OWN_TILES = [(0, 512, 0, 0), (512, 512, 0, 512), (1024, 128, 1, 2048)]


def na_chunk_plan():
    plan = []
    tid = 0
    shared = None
    for pl in range(8):
        lo0, hi0 = max(2 * pl - 4, 0), max(2 * pl - 3, 0) + 7
        lo1, hi1 = min(2 * pl - 4, 8), min(2 * pl - 3, 8) + 7
        lo, hi = min(lo0, lo1), max(hi0, hi1)
        chunks = list(range(lo // 2, hi // 2 + 1))
        if 2 <= pl <= 5:
            if shared is None:
                shared = list(range(tid, tid + len(chunks)))
                tid += len(chunks)
            tids = shared
        else:
            tids = list(range(tid, tid + len(chunks)))
            tid += len(chunks)
        plan.append([(c % 16, c, t) for c, t in zip(chunks, tids)])
    return plan, tid


NA_PLAN, NA_NT = na_chunk_plan()


def kcols(name, c, col0, n):
    return [(name, c, cb) for cb in range(col0 // 128, (col0 + n + 127) // 128)]


def segs(hh, ls, n):
    out = []
    bounds = [(0, 1024, hh * 1024), (1024, 2048, (1 - hh) * 1024),
              (2048, 2176, 2048 + hh * 128), (2176, 2304, 2048 + (1 - hh) * 128)]
    for (a, b, g) in bounds:
        lo, hi = max(ls, a), min(ls + n, b)
        if lo < hi:
            out.append((lo, g + lo - a, hi - lo))
    return out


def xd_keys(buf, gs, n):
    return [("Xd", buf, gb) for gb in range(gs // 128, (gs + n + 127) // 128)]


def fused_body(nc, P, es):
    def din(name, shape, dt=F32):
        return nc.dram_tensor(name, list(shape), dt, kind="ExternalInput").ap()

    uniq = [0]

    def sb(name, shape, dt, stack=None):
        uniq[0] += 1
        return (stack or es).enter_context(nc.sbuf_tensor("%s_%d" % (name, uniq[0]), list(shape), dt))

    x_in = din("x_in", [128, NCH, T_ALL])
    c2T = din("c2T", [128, NCH, 2])
    cst = din("cst", [128, 4, 128])
    fgT = din("fgT", [128, NCH])
    cosT_h = [din("cosT_h%d" % h, [128, SEQ]) for h in range(2)]
    sinT_h = [din("sinT_h%d" % h, [128, SEQ]) for h in range(2)]
    outT_all = nc.dram_tensor("outT", [2, 128, NCH, 1024], F32, kind="ExternalOutput").ap()
    Xs = [nc.dram_tensor("Xs%d" % i, [128, NCH, T_ALL], F32).ap() for i in range(2)]
    q_s = nc.dram_tensor("q_s", [16, 128, T_OWN], BF16).ap()
    k_s = nc.dram_tensor("k_s", [16, 128, T_ALL], BF16).ap()
    v_s = nc.dram_tensor("v_s", [T_ALL, D], BF16).ap()

    x_own = sb("x_own", [128, NCH, T_OWN], F32)
    B1 = sb("B1", [128, NCH, T_ALL], BF16)
    ones_bf = sb("ones_bf", [128, 128], BF16)
    ident_bf = sb("ident_bf", [128, 128], BF16)
    perm_bf = sb("perm_bf", [128, 128], BF16)
    ident_f = sb("ident_f", [128, 128], F32)
    GT = sb("GT", [NE, T_OWN], BF16)
    shared_mod = {"mod": sb("mod", [128, 96, 2], F32), "A1": sb("A1", [128, NCH, 2], F32), "A2": sb("A2", [128, NCH, 2], F32),
                  "gmix": sb("gmix", [128, NCH], F32), "gffn": sb("gffn", [128, NCH], F32)}
    ps = [es.enter_context(nc.psum_tensor("ps%d" % i, [128, 512], F32)) for i in range(8)]

    PS = lambda i: [("ps", i)]

    def dma_in(out_ap, in_ap, writes, cast=False, reads=()):
        q = "pool" if cast else "sp"
        return P.op(q, lambda e: e.dma_start(out=out_ap, in_=in_ap), reads=reads, writes=writes, dma=True)

    def dma_out(out_ap, in_ap, reads, writes=()):
        return P.op("sp", lambda e: e.dma_start(out=out_ap, in_=in_ap), reads=reads, writes=writes, dma=True)

    def mm(out_ap, lhsT, rhs, start, stop, reads, writes):
        return P.op("pe", lambda e: e.matmul(out_ap, lhsT=lhsT, rhs=rhs, start=start, stop=stop),
                    reads=reads, writes=writes)

    def act(out_ap, in_ap, func, reads, writes, bias=None, scale=None):
        kw = {}
        if bias is not None:
            kw["bias"] = bias
        if scale is not None:
            kw["scale"] = scale
        return P.op("act", lambda e: e.activation(out=out_ap, in_=in_ap, func=func, **kw), reads=reads, writes=writes)

    def dve(fn, reads, writes):
        return P.op("dve", fn, reads=reads, writes=writes)

    class Ring:
        def __init__(self, name, n, shape, dt, stack):
            self.t = [sb("%s%d" % (name, i), shape, dt, stack) for i in range(n)]
            self.k = [(name, i) for i in range(n)]
            self.i = 0

        def next(self):
            j = self.i % len(self.t)
            self.i += 1
            return self.t[j], [self.k[j]]

    dma_in(ones_bf[:], cst[:, 0, :], ["ones_bf"], cast=True)
    dma_in(ident_bf[:], cst[:, 1, :], ["ident_bf"], cast=True)
    dma_in(perm_bf[:], cst[:, 2, :], ["perm_bf"], cast=True)
    dma_in(ident_f[:], cst[:, 1, :], ["ident_f"])

    def declare_layer(li):
        kind = KINDS[li]
        nkh = 4 if kind == "gqa" else 16
        nvc = 4 if kind == "gqa" else 16
        nqkv = 16 + nkh + nvc
        s = "_%d" % li
        L = {}
        L["adaw"] = din("adaw" + s, [96, 128, NCH, 128])
        L["adabT"] = din("adabT" + s, [128, 96])
        L["gmixT"] = din("gmixT" + s, [128, NCH])
        L["gffnT"] = din("gffnT" + s, [128, NCH])
        L["wqkv"] = din("wqkv" + s, [nqkv, 128, NCH, 128])
        L["wo"] = din("wo" + s, [NCH, 128, NCH, 128])
        L["rw"] = din("rw" + s, [128, NCH, NE])
        L["rb"] = din("rb" + s, [1, NE])
        L["wg"] = din("wg" + s, [NE, NJ, 128, NCH, 128])
        L["wu"] = din("wu" + s, [NE, NJ, 128, NCH, 128])
        L["bgT"] = din("bgT" + s, [128, NE, NJ])
        L["buT"] = din("buT" + s, [128, NE, NJ])
        L["wd"] = din("wd" + s, [NE, 4, 128, NJ, 512])
        L["bd"] = din("bd" + s, [NE, D])
        if kind == "na":
            L["nabias"] = [din("nabias%s_h%d" % (s, h), [16, 128, NA_NT, 128]) for h in range(2)]
        if kind == "gqa":
            L["qkgT"] = din("qkgT" + s, [128, 2])
        if kind == "diff":
            L["lamT"] = din("lamT" + s, [128, 4, 128])
            L["sublnT"] = din("sublnT" + s, [128, 2])
        L.update(shared_mod)
        return L

    def emit_mods(li, L):
        adaw, adabT, mod, A1, A2, gmix, gffn = (L[k] for k in ("adaw", "adabT", "mod", "A1", "A2", "gmix", "gffn"))
        dma_in(gmix[:], L["gmixT"], ["gmix"])
        dma_in(gffn[:], L["gffnT"], ["gffn"])

        with contextlib.ExitStack() as ph:
            c2 = sb("c2", [128, NCH, 2], F32, ph)
            silu = sb("silu", [128, NCH, 2], BF16, ph)
            adab = sb("adab", [128, 96], F32, ph)
            wr = Ring("wrA", 4, [128, NCH, 128], BF16, ph)
            dma_in(c2[:], c2T, ["c2"])
            dma_in(adab[:], adabT, ["adab"])
            sg0 = sb("sg0", [128, NCH, 2], F32, ph)
            act(sg0[:], c2[:], AF.Sigmoid, ["c2"], ["sg0"])
            dve(lambda e: e.tensor_tensor(out=silu[:], in0=c2[:], in1=sg0[:], op=ALU.mult), ["c2", "sg0"], ["silu"])
            for ob in range(96):
                w, wk = wr.next()
                dma_in(w[:], adaw[ob], wk, cast=True)
                for c in range(NCH):
                    mm(ps[7][:, 2 * ob:2 * ob + 2], w[:, c, :], silu[:, c, :], c == 0, c == NCH - 1,
                       wk + ["silu"], PS(7))
            for v in range(2):
                dve(lambda e, v=v: e.tensor_tensor(out=mod[:, :, v], in0=ps[7][:, v:192:2], in1=adab[:], op=ALU.add),
                    PS(7) + ["adab"], ["mod"])
            for v in range(2):
                dve(lambda e, v=v: e.scalar_tensor_tensor(out=A1[:, :, v], in0=mod[:, 16:32, v], scalar=1.0, in1=gmix[:],
                                                          op0=ALU.add, op1=ALU.mult), ["mod", "gmix"], ["A1"])
                dve(lambda e, v=v: e.scalar_tensor_tensor(out=A2[:, :, v], in0=mod[:, 64:80, v], scalar=1.0, in1=gffn[:],
                                                          op0=ALU.add, op1=ALU.mult), ["mod", "gffn"], ["A2"])
            P.barrier()

    def emit_half(li, hh, L, mode):
        kind = KINDS[li]
        last = li == 3
        nqh = 16
        nkh = 4 if kind == "gqa" else 16
        nvc = 4 if kind == "gqa" else 16
        lam_init = 0.8 - 0.6 * math.exp(-0.3 * li)
        mod, A1, A2 = L["mod"], L["A1"], L["A2"]
        wqkv, wo, rw, rb, wg, wu, bgT, buT, wd, bd = (L[k] for k in ("wqkv", "wo", "rw", "rb", "wg", "wu", "bgT", "buT", "wd", "bd"))
        if kind == "na":
            nabias = L["nabias"][hh]
        if kind in ("gqa", "diff"):
            cosT, sinT = cosT_h[hh], sinT_h[hh]
        if kind == "gqa":
            qkgT = L["qkgT"]
        if kind == "diff":
            lamT, sublnT = L["lamT"], L["sublnT"]
        Xin, xin_id = (x_in, 2) if li == 0 else (Xs[(li - 1) % 2], (li - 1) % 2)
        Xout, xout_id = Xs[li % 2], li % 2
        outT = outT_all[hh]

        def load_x(dst_fn, ls, n, wkeys):
            for (lo, gs, ln) in segs(hh, ls, n):
                dma_in(dst_fn(lo - ls, ln), Xin[:, :, gs:gs + ln], wkeys, reads=xd_keys(xin_id, gs, ln))

        for (oc0, n, mc, ac0) in (OWN_TILES if mode == "half" else []):
            load_x(lambda r, ln, oc0=oc0: x_own[:, :, oc0 + r:oc0 + r + ln], ac0, n,
                   [k for c in range(NCH) for k in kcols("X", c, oc0, n)])

        def norm_mod(src, src_keys, dst, dst_keys, n, Amat, shift_seg, mc, tmp_ring, sq_ring, rs_t, bank):
            for c in range(NCH):
                sq, sqk = sq_ring.next()
                act(sq[:, :n], src(c), AF.Square, src_keys(c), sqk)
                mm(ps[bank][:, :n], ones_bf[:], sq[:, :n], c == 0, c == NCH - 1, sqk + ["ones_bf"], PS(bank))
            act(rs_t[:, :n], ps[bank][:, :n], AF.Sqrt, PS(bank), ["rs_t"], bias=EPS, scale=1.0 / D)
            dve(lambda e: e.reciprocal(out=rs_t[:, :n], in_=rs_t[:, :n]), ["rs_t"], ["rs_t"])
            for c in range(NCH):
                tmp, tk = tmp_ring.next()
                sc = src(c)
                dve(lambda e, c=c, tmp=tmp, sc=sc: e.scalar_tensor_tensor(out=tmp[:, :n], in0=sc, scalar=Amat[:, c, mc:mc + 1],
                                                                          in1=rs_t[:, :n], op0=ALU.mult, op1=ALU.mult),
                    src_keys(c) + ["rs_t", "A1", "A2"], tk)
                act(dst(c), tmp[:, :n], AF.Identity, tk + ["mod"], dst_keys(c),
                    bias=mod[:, shift_seg * 16 + c, mc:mc + 1], scale=1.0)

        with contextlib.ExitStack() as ph:
            xr = Ring("xt", 2, [128, NCH, 256], F32, ph)
            tmp_ring = Ring("tmpA", 3, [128, 256], F32, ph)
            sq_ring = Ring("sqA", 3, [128, 256], BF16, ph)
            rs_t = sb("rs_t", [128, 512], F32, ph)
            for (c0, n, mc) in (ALL_TILES if mode == "kv" else []):
                for s0 in range(c0, c0 + n, 256):
                    xt, xk = xr.next()
                    load_x(lambda r, ln, xt=xt: xt[:, :, r:r + ln], s0, 256, xk)
                    norm_mod(lambda c: xt[:, c, :], lambda c: xk,
                             lambda c: B1[:, c, s0:s0 + 256], lambda c: kcols("B1", c, s0, 256),
                             256, A1, 0, mc, tmp_ring, sq_ring, rs_t, 6)
            for (oc0, n, mc, ac0) in (OWN_TILES if mode == "half" else []):
                if last and mc == 1:
                    continue
                for s0 in range(oc0, oc0 + n, 256):
                    nn = min(256, oc0 + n - s0)
                    norm_mod(lambda c: x_own[:, c, s0:s0 + nn], lambda c: kcols("X", c, s0, nn),
                             lambda c: B1[:, c, s0:s0 + nn], lambda c: kcols("B1", c, s0, nn),
                             nn, A1, 0, mc, tmp_ring, sq_ring, rs_t, 6)
            P.barrier()

        rope = kind in ("gqa", "diff")
        with contextlib.ExitStack() as ph:
            wr = Ring("wrB", 3, [128, NCH, 128], BF16, ph)
            st_ring = Ring("stB", 4, [128, 512], BF16, ph)
            f_ring = Ring("fB", 6, [128, 512], F32, ph)
            stV = Ring("stV", 2, [128, 18, 128], BF16, ph)
            if rope:
                cos_sb = sb("cos_sb", [128, SEQ], BF16, ph)
                sin_sb = sb("sin_sb", [128, SEQ], BF16, ph)
                dma_in(cos_sb[:], cosT, ["cos_sb"], cast=True)
                dma_in(sin_sb[:], sinT, ["sin_sb"], cast=True)
            if kind == "gqa":
                qkg = sb("qkg", [128, 2], F32, ph)
                dma_in(qkg[:], qkgT, ["qkg"])
            bankc = [0]

            def nb(lo, cnt):
                bankc[0] += 1
                return lo + bankc[0] % cnt

            def do_rope(src_ap, src_keys, n, c0):
                st1, k1 = st_ring.next()
                act(st1[:, :n], src_ap, AF.Identity, src_keys, k1)
                pb = nb(2, 2)
                mm(ps[pb][:, :n], perm_bf[:], st1[:, :n], True, True, k1 + ["perm_bf"], PS(pb))
                f1, fk1 = f_ring.next()
                f2, fk2 = f_ring.next()
                dve(lambda e: e.tensor_tensor(out=f1[:, :n], in0=src_ap, in1=cos_sb[:, c0:c0 + n], op=ALU.mult),
                    src_keys + k1 + ["cos_sb"], fk1)
                dve(lambda e: e.tensor_tensor(out=f2[:, :n], in0=ps[pb][:, :n], in1=sin_sb[:, c0:c0 + n], op=ALU.mult),
                    PS(pb) + ["sin_sb"], fk2)
                st2, k2 = st_ring.next()
                dve(lambda e: e.tensor_tensor(out=st2[:, :n], in0=f1[:, :n], in1=f2[:, :n], op=ALU.add), fk1 + fk2, k2)
                return st2, k2

            def post_qk(pbank, n, is_q, lat, c0):
                src = ps[pbank][:, :n]
                sk = PS(pbank)
                if kind == "na":
                    st, k = st_ring.next()
                    act(st[:, :n], src, AF.Identity, sk, k, scale=(SCALE if is_q else 1.0))
                    return st, k
                if kind == "gqa":
                    sq, sqk = st_ring.next()
                    act(sq[:, :n], src, AF.Square, sk, sqk)
                    pb = nb(2, 2)
                    mm(ps[pb][:, :n], ones_bf[:], sq[:, :n], True, True, sqk + ["ones_bf"], PS(pb))
                    rs, rk = f_ring.next()
                    act(rs[:, :n], ps[pb][:, :n], AF.Sqrt, PS(pb), rk, bias=EPS, scale=1.0 / 128)
                    dve(lambda e: e.reciprocal(out=rs[:, :n], in_=rs[:, :n]), rk, rk)
                    qn, qnk = f_ring.next()
                    gi = 0 if is_q else 1
                    dve(lambda e: e.scalar_tensor_tensor(out=qn[:, :n], in0=src, scalar=qkg[:, gi:gi + 1], in1=rs[:, :n],
                                                         op0=ALU.mult, op1=ALU.mult), sk + rk + ["qkg"], qnk)
                    if lat:
                        return do_rope(qn[:, :n], qnk, n, c0)
                    st, k = st_ring.next()
                    act(st[:, :n], qn[:, :n], AF.Identity, qnk, k)
                    return st, k
                if lat:
                    return do_rope(src, sk, n, c0)
                st, k = st_ring.next()
                act(st[:, :n], src, AF.Identity, sk, k)
                return st, k

            for hq in (range(nqh) if mode == "half" else []):
                w, wk = wr.next()
                dma_in(w[:], wqkv[hq], wk, cast=True)
                for (oc0, n, mc, ac0) in OWN_TILES:
                    if last and mc == 1:
                        continue
                    pbk = nb(0, 2)
                    for c in range(NCH):
                        mm(ps[pbk][:, :n], w[:, c, :], B1[:, c, oc0:oc0 + n], c == 0, c == NCH - 1,
                           wk + kcols("B1", c, oc0, n), PS(pbk))
                    st, k = post_qk(pbk, n, True, mc == 0, ac0)
                    dma_out(q_s[hq][:, oc0:oc0 + n], st[:, :n], k, [("q_s", hq)])
            for hk in (range(nkh) if mode == "kv" else []):
                w, wk = wr.next()
                dma_in(w[:], wqkv[nqh + hk], wk, cast=True)
                for (c0, n, mc) in ALL_TILES:
                    pbk = nb(0, 2)
                    for c in range(NCH):
                        mm(ps[pbk][:, :n], w[:, c, :], B1[:, c, c0:c0 + n], c == 0, c == NCH - 1,
                           wk + kcols("B1", c, c0, n), PS(pbk))
                    st, k = post_qk(pbk, n, False, mc == 0, c0)
                    dma_out(k_s[hk][:, c0:c0 + n], st[:, :n], k, [("k_s", hk)])
            v_view = v_s.rearrange("(kc p) n -> p kc n", p=128)
            for vc in (range(nvc) if mode == "kv" else []):
                w, wk = wr.next()
                dma_in(w[:], wqkv[nqh + nkh + vc], wk, cast=True)
                sv, svk = stV.next()
                for g0 in range(0, 18, 4):
                    gn = min(4, 18 - g0)
                    pbk = nb(4, 2)
                    for gi in range(gn):
                        kc = g0 + gi
                        for c in range(NCH):
                            mm(ps[pbk][:, gi * 128:(gi + 1) * 128], B1[:, c, kc * 128:(kc + 1) * 128], w[:, c, :],
                               c == 0, c == NCH - 1, wk + kcols("B1", c, kc * 128, 128), PS(pbk))
                    act(sv[:, g0:g0 + gn, :], ps[pbk][:, :gn * 128].rearrange("p (a b) -> p a b", b=128), AF.Identity,
                        PS(pbk), svk)
                dma_out(v_view[:, :, vc * 128:(vc + 1) * 128], sv[:], svk, [("v_s", vc)])
            P.barrier()
        if mode == "kv":
            return

        with contextlib.ExitStack() as ph:
            dv = 256 if kind == "diff" else 128
            q_ring = Ring("qh", 2, [128, T_OWN], BF16, ph)
            k_ring = Ring("kh", 2, [128, T_ALL], BF16, ph)
            v_ring = Ring("vh", 2, [128, 18, dv], BF16, ph)
            e_ring = Ring("E", 3, [128, 512], BF16, ph)
            f_ring = Ring("fC", 8 if kind == "diff" else 3, [128, 512], F32, ph)
            if kind == "na":
                b_ring = Ring("nab", 2, [128, NA_NT, 128], BF16, ph)
            if kind == "diff":
                lam = sb("lam", [128, 4, 128], F32, ph)
                subln = sb("subln", [128, 2], F32, ph)
                lamw = sb("lamw", [128, 4], F32, ph)
                lamp = sb("lamp", [128, 2, 128], F32, ph)
                dma_in(lam[:], lamT, ["lam"])
                dma_in(subln[:], sublnT, ["subln"])
                dve(lambda e: e.tensor_tensor(out=lamp[:, 0, :], in0=lam[:, 0, :], in1=lam[:, 1, :], op=ALU.mult), ["lam"], ["lamp"])
                dve(lambda e: e.tensor_tensor(out=lamp[:, 1, :], in0=lam[:, 2, :], in1=lam[:, 3, :], op=ALU.mult), ["lam"], ["lamp"])
                dve(lambda e: e.reduce_sum(out=lamw[:, 0:1], in_=lamp[:, 0, :], axis=AX.X), ["lamp"], ["lamw"])
                dve(lambda e: e.reduce_sum(out=lamw[:, 1:2], in_=lamp[:, 1, :], axis=AX.X), ["lamp"], ["lamw"])
                act(lamw[:, 2:4], lamw[:, 0:2], AF.Exp, ["lamw"], ["lamw"])
                dve(lambda e: e.scalar_tensor_tensor(out=lamw[:, 0:1], in0=lamw[:, 3:4], scalar=-lam_init, in1=lamw[:, 2:3],
                                                     op0=ALU.add, op1=ALU.subtract), ["lamw"], ["lamw"])
                dve(lambda e: e.tensor_scalar(out=subln[:], in0=subln[:], scalar1=1.0 - lam_init, scalar2=None, op0=ALU.mult),
                    ["subln"], ["subln"])
            cnt = {"s": 0, "o": 0, "d": 0}

            def bank(kind_, lo, n_):
                cnt[kind_] += 1
                return lo + cnt[kind_] % n_

            def attend(qt, qk, kt, kk, vt, vk, q0, n, chunks, ndv, bias=None, bk=None):
                pO = [bank("o", 2, 4) for _ in range(ndv)]
                pD = bank("d", 6, 2)
                nchk = len(chunks)
                for i, (kc, tid) in enumerate(chunks):
                    sbk = bank("s", 0, 2)
                    mm(ps[sbk][:, :n], kt[:, kc * 128:(kc + 1) * 128], qt[:, q0:q0 + n], True, tid is None, kk + qk, PS(sbk))
                    if tid is not None:
                        mm(ps[sbk][:, :n], ident_bf[:], bias[:, tid, :], False, True, bk + ["ident_bf"], PS(sbk))
                    E, ek = e_ring.next()
                    act(E[:, :n], ps[sbk][:, :n], AF.Exp, PS(sbk), ek, scale=(1.0 if kind == "na" else SCALE))
                    for dvc in range(ndv):
                        mm(ps[pO[dvc]][:, :n], vt[:, kc, dvc * 128:(dvc + 1) * 128], E[:, :n], i == 0, i == nchk - 1,
                           vk + ek, PS(pO[dvc]))
                    mm(ps[pD][:, :n], ones_bf[:], E[:, :n], i == 0, i == nchk - 1, ek + ["ones_bf"], PS(pD))
                rd, rk = f_ring.next()
                dve(lambda e: e.reciprocal(out=rd[:, :n], in_=ps[pD][:, :n]), PS(pD), rk)
                return pO, rd, rk

            def gch(kc):
                if hh == 0 or kc >= 16:
                    return kc
                return (kc + 8) % 16

            def q_tiles():
                ctxq = [] if last else [(1024, 128, [(16, None), (17, None)])]
                if kind == "na":
                    return [(pl * 128, 128, [(16, None), (17, None)] + [(gch(kc), tid) for (kc, _, tid) in NA_PLAN[pl]])
                            for pl in range(8)] + ctxq
                allc = [(kc, None) for kc in range(18)]
                return [(0, 512, allc), (512, 512, allc)] + ctxq

            def load_head(hq):
                qt, qk = q_ring.next()
                kt, kk = k_ring.next()
                dma_in(qt[:], q_s[hq], qk, reads=[("q_s", hq)])
                hk = hq // 4 if kind == "gqa" else hq
                dma_in(kt[:], k_s[hk], kk, reads=[("k_s", hk)])
                return qt, qk, kt, kk

            if kind != "diff":
                for hq in range(16):
                    qt, qk, kt, kk = load_head(hq)
                    vt, vk = v_ring.next()
                    vcol = (hq // 4 if kind == "gqa" else hq) * 128
                    dma_in(vt[:], v_view[:, :, vcol:vcol + 128], vk, reads=[("v_s", vcol // 128)])
                    bias = bk = None
                    if kind == "na":
                        bias, bk = b_ring.next()
                        dma_in(bias[:], nabias[hq], bk, cast=True)
                    for (q0, n, chunks) in q_tiles():
                        pO, rd, rk = attend(qt, qk, kt, kk, vt, vk, q0, n, chunks, 1, bias, bk)
                        dve(lambda e, pO=pO, rd=rd, q0=q0, n=n, hq=hq: e.tensor_tensor(
                            out=B1[:, hq, q0:q0 + n], in0=ps[pO[0]][:, :n], in1=rd[:, :n], op=ALU.mult),
                            PS(pO[0]) + rk, kcols("B1", hq, q0, n))
            else:
                for hd in range(8):
                    qa = load_head(2 * hd)
                    vt, vk = v_ring.next()
                    dma_in(vt[:], v_view[:, :, hd * 256:(hd + 1) * 256], vk, reads=[("v_s", 2 * hd), ("v_s", 2 * hd + 1)])
                    qb = load_head(2 * hd + 1)
                    for (q0, n, chunks) in q_tiles():
                        pO, rd, rk = attend(qa[0], qa[1], qa[2], qa[3], vt, vk, q0, n, chunks, 2)
                        o0 = []
                        for dvc in range(2):
                            f, fk = f_ring.next()
                            dve(lambda e, f=f, p=pO[dvc], rd=rd, n=n: e.tensor_tensor(out=f[:, :n], in0=ps[p][:, :n], in1=rd[:, :n],
                                                                                     op=ALU.mult), PS(pO[dvc]) + rk, fk)
                            o0.append((f, fk))
                        pO, rd, rk = attend(qb[0], qb[1], qb[2], qb[3], vt, vk, q0, n, chunks, 2)
                        od = []
                        sbk = bank("s", 0, 2)
                        for dvc in range(2):
                            f, fk = f_ring.next()
                            dve(lambda e, f=f, p=pO[dvc], rd=rd, n=n: e.tensor_tensor(out=f[:, :n], in0=ps[p][:, :n], in1=rd[:, :n],
                                                                                     op=ALU.mult), PS(pO[dvc]) + rk, fk)
                            dve(lambda e, f=f, o=o0[dvc][0], n=n: e.scalar_tensor_tensor(out=f[:, :n], in0=f[:, :n], scalar=lamw[:, 0:1],
                                                                                          in1=o[:, :n], op0=ALU.mult, op1=ALU.add),
                                fk + o0[dvc][1] + ["lamw"], fk)
                            od.append((f, fk))
                            E, ek = e_ring.next()
                            act(E[:, :n], f[:, :n], AF.Square, fk, ek)
                            mm(ps[sbk][:, :n], ones_bf[:], E[:, :n], dvc == 0, dvc == 1, ek + ["ones_bf"], PS(sbk))
                        rs, rsk = f_ring.next()
                        act(rs[:, :n], ps[sbk][:, :n], AF.Sqrt, PS(sbk), rsk, bias=EPS, scale=1.0 / 256)
                        dve(lambda e, rs=rs, n=n: e.reciprocal(out=rs[:, :n], in_=rs[:, :n]), rsk, rsk)
                        for dvc in range(2):
                            hidx = 2 * hd + dvc
                            dve(lambda e, f=od[dvc][0], rs=rs, n=n, q0=q0, hidx=hidx, dvc=dvc: e.scalar_tensor_tensor(
                                out=B1[:, hidx, q0:q0 + n], in0=f[:, :n], scalar=subln[:, dvc:dvc + 1], in1=rs[:, :n],
                                op0=ALU.mult, op1=ALU.mult), od[dvc][1] + rsk + ["subln"], kcols("B1", hidx, q0, n))
            P.barrier()

        own_tiles = [t for t in OWN_TILES if not (last and t[2] == 1)]
        HO = T_OWN
        with contextlib.ExitStack() as ph:
            wr = Ring("wrD", 3, [128, NCH, 128], BF16, ph)
            tmp_ring = Ring("tmpD", 3, [128, 256], F32, ph)
            sq_ring = Ring("sqD", 3, [128, 256], BF16, ph)
            rs_t2 = sb("rs_t", [128, 512], F32, ph)
            rw_bf = sb("rw_bf", [128, NCH, NE], BF16, ph)
            rb_bf = sb("rb_bf", [1, NE], BF16, ph)
            r_ring = Ring("rt", 2, [128, 6, NE], F32, ph)
            dma_in(rw_bf[:], rw, ["rw_bf"], cast=True)
            dma_in(rb_bf[:], rb, ["rb_bf"], cast=True)
            bc = 0
            for fc in range(NCH):
                w, wk = wr.next()
                dma_in(w[:], wo[fc], wk, cast=True)
                for (oc0, n, mc, ac0) in own_tiles:
                    bc += 1
                    b = bc % 2
                    for h in range(NCH):
                        mm(ps[b][:, :n], w[:, h, :], B1[:, h, oc0:oc0 + n], h == 0, h == NCH - 1,
                           wk + kcols("B1", h, oc0, n), PS(b))
                    xk = kcols("X", fc, oc0, n)
                    dve(lambda e, b=b, n=n, fc=fc, oc0=oc0, mc=mc: e.scalar_tensor_tensor(
                        out=x_own[:, fc, oc0:oc0 + n], in0=ps[b][:, :n], scalar=mod[:, 32 + fc, mc:mc + 1],
                        in1=x_own[:, fc, oc0:oc0 + n], op0=ALU.mult, op1=ALU.add), PS(b) + xk + ["mod"], xk)
            for (oc0, n, mc, ac0) in own_tiles:
                for s0 in range(oc0, oc0 + n, 256):
                    nn = min(256, oc0 + n - s0)
                    norm_mod(lambda c: x_own[:, c, s0:s0 + nn], lambda c: kcols("X", c, s0, nn),
                             lambda c: B1[:, c, HO + s0:HO + s0 + nn], lambda c: kcols("B1", c, HO + s0, nn),
                             nn, A2, 3, mc, tmp_ring, sq_ring, rs_t2, 6)
            nblk = sum(t[1] for t in own_tiles) // 128
            for blk in range(nblk):
                b = 2 + blk % 2
                c0 = HO + blk * 128
                for c in range(NCH):
                    mm(ps[b][:, 0:NE], B1[:, c, c0:c0 + 128], rw_bf[:, c, :], c == 0, False,
                       kcols("B1", c, c0, 128) + ["rw_bf"], PS(b))
                mm(ps[b][:, 0:NE], ones_bf[0:1, :], rb_bf[0:1, :], False, True, ["ones_bf", "rb_bf"], PS(b))
                rt, rk = r_ring.next()
                lg, t8, mk, ex, sm, G = (rt[:, 0, :], rt[:, 1, :], rt[:, 2, :], rt[:, 3, :], rt[:, 4, :], rt[:, 5, :])
                dve(lambda e, lg=lg, b=b: e.tensor_copy(out=lg, in_=ps[b][:, 0:NE]), PS(b), rk)
                dve(lambda e, lg=lg, t8=t8: e.max(out=t8[:, 0:8], in_=lg), rk, rk)
                dve(lambda e, lg=lg, t8=t8, mk=mk: e.tensor_scalar(out=mk, in0=lg, scalar1=t8[:, 3:4], scalar2=None, op0=ALU.is_ge),
                    rk, rk)
                dve(lambda e, t8=t8, sm=sm: e.tensor_scalar(out=sm[:, 0:1], in0=t8[:, 0:1], scalar1=-1.0, scalar2=None, op0=ALU.mult),
                    rk, rk)
                act(ex, lg, AF.Exp, rk, rk, bias=sm[:, 0:1], scale=1.0)
                dve(lambda e, ex=ex, mk=mk: e.tensor_tensor(out=ex, in0=ex, in1=mk, op=ALU.mult), rk, rk)
                dve(lambda e, ex=ex, sm=sm: e.reduce_sum(out=sm[:, 1:2], in_=ex, axis=AX.X), rk, rk)
                dve(lambda e, sm=sm: e.reciprocal(out=sm[:, 2:3], in_=sm[:, 1:2]), rk, rk)
                dve(lambda e, ex=ex, sm=sm, G=G: e.tensor_scalar(out=G, in0=ex, scalar1=sm[:, 2:3], scalar2=None, op0=ALU.mult), rk, rk)
                b2 = 4 + blk % 2
                P.op("pe", lambda e, G=G, b2=b2: e.transpose(out=ps[b2][0:NE, 0:128], in_=G, identity=ident_f[:]),
                     reads=rk + ["ident_f"], writes=PS(b2))
                act(GT[0:NE, blk * 128:(blk + 1) * 128], ps[b2][0:NE, 0:128], AF.Identity, PS(b2), [("GT", blk)])
            P.barrier()

        with contextlib.ExitStack() as ph:
            wr = Ring("wrE", 3, [128, NCH, 128], BF16, ph)
            wdr = Ring("wdE", 2, [128, NJ, 512], BF16, ph)
            actG = sb("actG", [128, NJ, T_OWN], BF16, ph)
            gte_r = Ring("GTe", 1, [NE, T_OWN], BF16, ph)
            gb_r = Ring("Gb", 1, [128, T_OWN], BF16, ph)
            gf_r = Ring("gf", 2, [128, 512], F32, ph)
            sg_r = Ring("sgE", 2, [128, 512], BF16, ph)
            uf_r = Ring("uf", 2, [128, 512], F32, ph)
            bg = sb("bg", [128, NE, NJ], F32, ph)
            bu = sb("bu", [128, NE, NJ], F32, ph)
            bd_bf = sb("bd_bf", [NE, D], BF16, ph)
            dma_in(bg[:], bgT, ["bg"])
            dma_in(bu[:], buT, ["bu"])
            dma_in(bd_bf[:], bd, ["bd_bf"], cast=True)
            GTk = [("GT", blk) for blk in range(9)]
            ncols = sum(t[1] for t in own_tiles)
            pc = 0
            for fc in range(NCH):
                for (oc0, n, mc, ac0) in own_tiles:
                    pc += 1
                    b = pc % 2
                    mm(ps[b][:, :n], bd_bf[0:NE, fc * 128:(fc + 1) * 128], GT[0:NE, oc0:oc0 + n], True, True,
                       ["bd_bf"] + GTk, PS(b))
                    xk = kcols("X", fc, oc0, n)
                    dve(lambda e, b=b, n=n, fc=fc, oc0=oc0, mc=mc: e.scalar_tensor_tensor(
                        out=x_own[:, fc, oc0:oc0 + n], in0=ps[b][:, :n], scalar=mod[:, 80 + fc, mc:mc + 1],
                        in1=x_own[:, fc, oc0:oc0 + n], op0=ALU.mult, op1=ALU.add), PS(b) + xk + ["mod"], xk)
            for ex in range(NE):
                gte, gtek = gte_r.next()
                dve(lambda e, gte=gte, ex=ex: e.tensor_scalar(out=gte[0:NE, 0:ncols], in0=GT[0:NE, 0:ncols],
                                                              scalar1=ident_f[0:NE, ex:ex + 1], scalar2=None, op0=ALU.mult),
                    GTk + ["ident_f"], gtek)
                gb, gbk = gb_r.next()
                for (oc0, n, mc, ac0) in own_tiles:
                    pc += 1
                    b = pc % 2
                    mm(ps[b][:, :n], ones_bf[0:NE, :], gte[0:NE, oc0:oc0 + n], True, True, gtek + ["ones_bf"], PS(b))
                    act(gb[:, oc0:oc0 + n], ps[b][:, :n], AF.Identity, PS(b), gbk)
                for j in range(NJ):
                    wgb, wgk = wr.next()
                    dma_in(wgb[:], wg[ex, j], wgk, cast=True)
                    wub, wuk = wr.next()
                    dma_in(wub[:], wu[ex, j], wuk, cast=True)
                    for (oc0, n, mc, ac0) in own_tiles:
                        pc += 1
                        pg = 2 * (pc % 2)
                        pu = pg + 1
                        for c in range(NCH):
                            mm(ps[pg][:, :n], wgb[:, c, :], B1[:, c, HO + oc0:HO + oc0 + n], c == 0, c == NCH - 1,
                               wgk + kcols("B1", c, HO + oc0, n), PS(pg))
                        for c in range(NCH):
                            mm(ps[pu][:, :n], wub[:, c, :], B1[:, c, HO + oc0:HO + oc0 + n], c == 0, c == NCH - 1,
                               wuk + kcols("B1", c, HO + oc0, n), PS(pu))
                        gf, gfk = gf_r.next()
                        sg, sgk = sg_r.next()
                        uf, ufk = uf_r.next()
                        dve(lambda e, gf=gf, pg=pg, n=n, ex=ex, j=j: e.tensor_scalar(
                            out=gf[:, :n], in0=ps[pg][:, :n], scalar1=bg[:, ex, j:j + 1], scalar2=7.0, op0=ALU.add, op1=ALU.min),
                            PS(pg) + ["bg"], gfk)
                        act(sg[:, :n], gf[:, :n], AF.Sigmoid, gfk, sgk, scale=1.702)
                        dve(lambda e, uf=uf, pu=pu, n=n, ex=ex, j=j: e.tensor_scalar(
                            out=uf[:, :n], in0=ps[pu][:, :n], scalar1=bu[:, ex, j:j + 1], scalar2=7.0, op0=ALU.add, op1=ALU.min),
                            PS(pu) + ["bu"], ufk)
                        dve(lambda e, uf=uf, n=n: e.tensor_scalar(out=uf[:, :n], in0=uf[:, :n], scalar1=-7.0, scalar2=1.0,
                                                                  op0=ALU.max, op1=ALU.add), ufk, ufk)
                        dve(lambda e, gf=gf, sg=sg, n=n: e.tensor_tensor(out=gf[:, :n], in0=gf[:, :n], in1=sg[:, :n], op=ALU.mult),
                            gfk + sgk, gfk)
                        dve(lambda e, gf=gf, uf=uf, n=n: e.tensor_tensor(out=gf[:, :n], in0=gf[:, :n], in1=uf[:, :n], op=ALU.mult),
                            gfk + ufk, gfk)
                        ak = kcols("AG", j, oc0, n)
                        dve(lambda e, gf=gf, gb=gb, n=n, j=j, oc0=oc0: e.tensor_tensor(
                            out=actG[:, j, oc0:oc0 + n], in0=gf[:, :n], in1=gb[:, oc0:oc0 + n], op=ALU.mult), gfk + gbk, ak)
                for grp in range(4):
                    wdb, wdk = wdr.next()
                    dma_in(wdb[:], wd[ex, grp], wdk, cast=True)
                    for (oc0, n, mc, ac0) in own_tiles:
                        for f4 in range(4):
                            fc = grp * 4 + f4
                            pc += 1
                            b = 4 + pc % 4
                            for j in range(NJ):
                                mm(ps[b][:, :n], wdb[:, j, f4 * 128:(f4 + 1) * 128], actG[:, j, oc0:oc0 + n], j == 0, j == NJ - 1,
                                   wdk + kcols("AG", j, oc0, n), PS(b))
                            xk = kcols("X", fc, oc0, n)
                            dve(lambda e, b=b, n=n, fc=fc, oc0=oc0, mc=mc: e.scalar_tensor_tensor(
                                out=x_own[:, fc, oc0:oc0 + n], in0=ps[b][:, :n], scalar=mod[:, 80 + fc, mc:mc + 1],
                                in1=x_own[:, fc, oc0:oc0 + n], op0=ALU.mult, op1=ALU.add), PS(b) + xk + ["mod"], xk)
            P.barrier()

        if not last:
            for (oc0, n, mc, ac0) in OWN_TILES:
                for (lo, gs, ln) in segs(hh, ac0, n):
                    dma_out(Xout[:, :, gs:gs + ln], x_own[:, :, oc0 + lo - ac0:oc0 + lo - ac0 + ln],
                            [k for c in range(NCH) for k in kcols("X", c, oc0, n)], xd_keys(xout_id, gs, ln))
        else:
            with contextlib.ExitStack() as ph:
                fg = sb("fg", [128, NCH], F32, ph)
                dma_in(fg[:], fgT, ["fg"])
                sq_ring = Ring("sqF", 3, [128, 256], BF16, ph)
                o_ring = Ring("oF", 2, [128, NCH, 256], F32, ph)
                rs_f = sb("rs_f", [128, 256], F32, ph)
                for s0 in range(0, 1024, 256):
                    for c in range(NCH):
                        sq, sqk = sq_ring.next()
                        act(sq[:], x_own[:, c, s0:s0 + 256], AF.Square, kcols("X", c, s0, 256), sqk)
                        mm(ps[0][:, :256], ones_bf[:], sq[:], c == 0, c == NCH - 1, sqk + ["ones_bf"], PS(0))
                    act(rs_f[:], ps[0][:, :256], AF.Sqrt, PS(0), ["rs_f"], bias=EPS, scale=1.0 / D)
                    dve(lambda e: e.reciprocal(out=rs_f[:], in_=rs_f[:]), ["rs_f"], ["rs_f"])
                    ot, ok_ = o_ring.next()
                    for c in range(NCH):
                        dve(lambda e, c=c, ot=ot, s0=s0: e.scalar_tensor_tensor(
                            out=ot[:, c, :], in0=x_own[:, c, s0:s0 + 256], scalar=fg[:, c:c + 1], in1=rs_f[:],
                            op0=ALU.mult, op1=ALU.mult), kcols("X", c, s0, 256) + ["rs_f", "fg"], ok_)
                    dma_out(outT[:, :, s0:s0 + 256], ot[:], ok_)


    for li in range(4):
        L = declare_layer(li)
        emit_mods(li, L)
        emit_half(li, 0, L, "kv")
        for hh in range(2):
            emit_half(li, hh, L, "half")


_PROG_CACHE = {}


def get_prog():
    if "f" not in _PROG_CACHE:
        _PROG_CACHE["f"] = build_and_emit(fused_body)
    return _PROG_CACHE["f"]


def _blk(W):
    K, N = W.shape
    return np.ascontiguousarray(W.reshape(K // 128, 128, N // 128, 128).transpose(2, 1, 0, 3))


def _fm(v, n):
    return np.ascontiguousarray(v.reshape(n, 128).T)


def _consts():
    cst = np.zeros((128, 4, 128), np.float32)
    cst[:, 0, :] = 1.0
    cst[:, 1, :] = np.eye(128, dtype=np.float32)
    idx = np.arange(128)
    cst[idx ^ 32, 2, idx] = 1.0
    return cst


def _rope_tables(h):
    t = (np.arange(SEQ) + 1024 * h) % SEQ
    row = (t // 64).astype(np.float32)
    col = (t % 64).astype(np.float32)
    half = 64
    inv_freq = (np.float32(10000.0) ** (-np.arange(0, half, 2, dtype=np.float32) / np.float32(half))).astype(np.float32)
    ang_r = row[:, None] * inv_freq[None, :]
    ang_c = col[:, None] * inv_freq[None, :]
    ang = np.concatenate([ang_r, ang_r, ang_c, ang_c], axis=-1)
    cos = np.cos(ang).astype(np.float32)
    sin = np.sin(ang).astype(np.float32)
    sgn = np.ones(128, np.float32)
    sgn[0:32] = -1.0
    sgn[64:96] = -1.0
    return np.ascontiguousarray(cos.T), np.ascontiguousarray((sin * sgn[None, :]).T)


def _na_bias(rpb, h):
    out = np.full((16, 128, NA_NT, 128), -30000.0, np.float32)
    ki = np.arange(128)[:, None]
    qi = np.arange(128)[None, :]
    for pl in range(8):
        for (kcm, cl, tid) in NA_PLAN[pl]:
            kr = 2 * cl + ki // 64
            kc_ = ki % 64
            ql = 2 * pl + qi // 64
            qc = qi % 64
            r = 16 * h + ql
            rp = 16 * h + kr
            r0 = np.clip(r - 4, 0, 24)
            cs = np.clip(qc - 8, 0, 48)
            valid = (rp >= 0) & (rp <= 31) & (rp >= r0) & (rp < r0 + 8) & (kc_ >= cs) & (kc_ < cs + 16)
            dri = np.clip(rp - r + 7, 0, 14)
            dci = np.clip(kc_ - qc + 15, 0, 30)
            vals = rpb[:, dri, dci]
            out[:, :, tid, :] = np.where(valid[None], vals, np.float32(-30000.0))
    return out


def _layer_inputs(inp, li):
    kind = KINDS[li]
    j = li // 3
    sfx = "_%d" % li
    m = {}
    m["adaw"] = _blk(inp["ada_w"][li])
    m["adabT"] = _fm(inp["ada_b"][li], 96)
    m["gmixT"] = _fm(inp["norm_mix_g"][li], 16)
    m["gffnT"] = _fm(inp["norm_ffn_g"][li], 16)
    if kind == "na":
        m["wqkv"] = _blk(inp["na_wqkv"][j])
        m["wo"] = _blk(inp["na_wo"][j])
    elif kind == "gqa":
        m["wqkv"] = _blk(inp["gqa_wqkv"][j])
        m["wo"] = _blk(inp["gqa_wo"][j])
        m["qkgT"] = np.ascontiguousarray(np.stack([inp["gqa_q_g"][j], inp["gqa_k_g"][j]], axis=1))
    else:
        m["wqkv"] = _blk(inp["diff_wqkv"][j])
        m["wo"] = _blk(inp["diff_wo"][j])
        m["lamT"] = np.ascontiguousarray(np.broadcast_to(inp["diff_lambda"][j][None], (128, 4, 128)))
        m["sublnT"] = _fm(inp["diff_subln_g"][j], 2)
    m["rw"] = np.ascontiguousarray(inp["router_w"][li].reshape(16, 128, NE).transpose(1, 0, 2))
    m["rb"] = np.ascontiguousarray(inp["router_b"][li].reshape(1, NE))
    m["wg"] = np.ascontiguousarray(inp["w_gate"][li].reshape(NE, 16, 128, NJ, 128).transpose(0, 3, 2, 1, 4))
    m["wu"] = np.ascontiguousarray(inp["w_up"][li].reshape(NE, 16, 128, NJ, 128).transpose(0, 3, 2, 1, 4))
    m["bgT"] = np.ascontiguousarray(inp["b_gate"][li].reshape(NE, NJ, 128).transpose(2, 0, 1))
    m["buT"] = np.ascontiguousarray(inp["b_up"][li].reshape(NE, NJ, 128).transpose(2, 0, 1))
    m["wd"] = np.ascontiguousarray(inp["w_down"][li].reshape(NE, NJ, 128, 4, 512).transpose(0, 3, 2, 1, 4))
    m["bd"] = np.ascontiguousarray(inp["b_down"][li])
    out = {k + sfx: v for k, v in m.items()}
    if kind == "na":
        for h in range(2):
            out["nabias%s_h%d" % (sfx, h)] = _na_bias(inp["na_rpb"][j], h)
    return out


def _to_fm(a):
    T = a.shape[0]
    return np.ascontiguousarray(a.T.reshape(16, 128, T).transpose(1, 0, 2))


def kernel(**inp):
    inp = {k: np.asarray(v) for k, v in inp.items()}
    nc = get_prog()
    shared = {"cst": _consts(), "fgT": _fm(inp["final_g"], 16)}
    for h in range(2):
        shared["cosT_h%d" % h], shared["sinT_h%d" % h] = _rope_tables(h)
    for li in range(4):
        shared.update(_layer_inputs(inp, li))
    in_maps = []
    per_b = {}
    for core in range(8):
        b = core // 2
        if b not in per_b:
            c2 = np.stack([inp["c"][b], inp["c_ctx"]], axis=0)
            per_b[b] = {
                "x_in": np.concatenate([_to_fm(inp["x"][b]), _to_fm(inp["ctx"][b])], axis=2),
                "c2T": np.ascontiguousarray(c2.reshape(2, 16, 128).transpose(2, 1, 0)),
            }
        m = dict(shared)
        m.update(per_b[b])
        in_maps.append(m)
    res = run_bass_kernel_spmd(nc, in_maps, core_ids=list(range(8)))
    outs = res.results
    out = np.empty((4, SEQ, D), np.float32)
    for b in range(4):
        o = outs[2 * b]["outT"]
        for h in range(2):
            out[b, 1024 * h:1024 * (h + 1), :] = o[h].transpose(2, 1, 0).reshape(1024, D)
    return out
```
